# Optimizing a Trainium2 kernel written in Bass

```python
import math
import jax, jax.numpy as jnp
from jax import lax
import numpy as np


D_MODEL = 1024
BATCH = 8
SEQ = 8192
DEPTH = 2

ATTN_HEADS = 8
ATTN_KV_GROUPS = 2
HEAD_DIM = 64
IDX_HEADS = 4
IDX_DIM = 64
TOPK_MAX = 256
Q_BLOCK = 128
NUM_BUCKETS = 32
MAX_DISTANCE = 128
HGRN_HEADS = 4
HGRN_DIM = 128
HGRN_CHUNK = 64
CONV_W = 512
CONV_K = 3
D_FF = 4 * D_MODEL

ATTN_W = ATTN_HEADS * HEAD_DIM
KV_W = ATTN_KV_GROUPS * HEAD_DIM
HGRN_W = HGRN_HEADS * HGRN_DIM
N_BRANCH = 3
EPS = 1e-6
SPLITS = (ATTN_W, KV_W, KV_W,
          IDX_HEADS * IDX_DIM, IDX_DIM, IDX_HEADS,
          HGRN_W, HGRN_W, HGRN_W, HGRN_W,
          CONV_W, CONV_W, CONV_W,
          N_BRANCH * D_MODEL)
D_IN_PROJ = sum(SPLITS)

kernel_name = "hybrid_dsa_hgrn2_shortconv_block"


def _rmsnorm(x, g):
    xf = x.astype(jnp.float32)
    y = xf * lax.rsqrt(jnp.mean(xf * xf, axis=-1, keepdims=True) + EPS)
    return (y * g.astype(jnp.float32)).astype(x.dtype)


def _split_cols(p):
    offs, acc = [], 0
    for w in SPLITS[:-1]:
        acc += w
        offs.append(acc)
    return jnp.split(p, offs, axis=-1)


def _t5_bucket(dist):
    max_exact = NUM_BUCKETS // 2
    d = jnp.maximum(dist, 0)
    df = jnp.maximum(d, 1).astype(jnp.float32)
    large = max_exact + (jnp.log(df / max_exact) / math.log(MAX_DISTANCE / max_exact)
                         * (NUM_BUCKETS - max_exact)).astype(jnp.int32)
    large = jnp.minimum(large, NUM_BUCKETS - 1)
    return jnp.where(d < max_exact, d, large)


def _dsa_attention(q, k, v, qi, ki, wi, rel_bias):
    b, s = q.shape[0], q.shape[1]
    n_sel = min(TOPK_MAX, s // 4)
    nb = s // Q_BLOCK
    npg = ATTN_HEADS // ATTN_KV_GROUPS
    scale = HEAD_DIM ** -0.5
    wi = wi * (IDX_HEADS ** -0.5 * IDX_DIM ** -0.5)
    key_pos = jnp.arange(s)
    bidx = jnp.arange(b)[:, None, None]

    def blocks(a):
        return jnp.moveaxis(a.reshape((b, nb, Q_BLOCK) + a.shape[2:]), 1, 0)

    def one_block(args):
        blk, q_b, qi_b, wi_b = args
        t = blk * Q_BLOCK + jnp.arange(Q_BLOCK)
        sc = jax.nn.relu(jnp.einsum('bqhd,bsd->bqhs', qi_b, ki))
        sc = jnp.einsum('bqhs,bqh->bqs', sc, wi_b).astype(jnp.float32)
        causal = key_pos[None, :] <= t[:, None]
        sc = jnp.where(causal[None], sc, -jnp.inf)
        _, idx = lax.top_k(sc, n_sel)
        valid = idx <= t[None, :, None]
        k_sel = k[bidx, idx]
        v_sel = v[bidx, idx]
        qg = q_b.reshape(b, Q_BLOCK, ATTN_KV_GROUPS, npg, HEAD_DIM)
        logits = jnp.einsum('bqgnd,bqkgd->bqgnk', qg, k_sel).astype(jnp.float32) * scale
        bias = rel_bias[_t5_bucket(t[None, :, None] - idx)]
        bias = jnp.moveaxis(bias, -1, 2).reshape(b, Q_BLOCK, ATTN_KV_GROUPS, npg, n_sel)
        logits = jnp.where(valid[:, :, None, None, :], logits + bias.astype(jnp.float32), -jnp.inf)
        p = jax.nn.softmax(logits, axis=-1).astype(v.dtype)
        o = jnp.einsum('bqgnk,bqkgd->bqgnd', p, v_sel)
        return o.reshape(b, Q_BLOCK, ATTN_W)

    out = lax.map(one_block, (jnp.arange(nb), blocks(q), blocks(qi), blocks(wi)))
    return jnp.moveaxis(out, 0, 1).reshape(b, s, ATTN_W)


def _hgrn2(q, f_logit, i, lb):
    b, s = q.shape[0], q.shape[1]
    nc = s // HGRN_CHUNK
    f32 = jnp.float32
    z = f_logit.astype(f32)
    lb = lb.astype(f32).reshape(HGRN_HEADS, HGRN_DIM)
    log_f = jnp.logaddexp(jnp.log(lb), jnp.log1p(-lb) + jax.nn.log_sigmoid(z))
    kk = (1.0 - lb) * jax.nn.sigmoid(-z)

    def chunks(a):
        return a.reshape(b, nc, HGRN_CHUNK, HGRN_HEADS, HGRN_DIM).transpose(1, 0, 3, 2, 4)

    tri = jnp.tril(jnp.ones((HGRN_CHUNK, HGRN_CHUNK), dtype=bool))

    def step(state, inp):
        qc, kc, ic, gc = inp
        G = jnp.cumsum(gc, axis=2)
        diff = G[:, :, :, None, :] - G[:, :, None, :, :]
        decay = jnp.exp(jnp.where(tri[:, :, None], diff, -jnp.inf))
        attn = jnp.einsum('bhtd,bhsd,bhtsd->bhts', qc, kc, decay)
        o = attn @ ic + jnp.einsum('bhtd,bhdv->bhtv', qc * jnp.exp(G), state)
        g_last = G[:, :, -1:, :]
        state = (jnp.exp(g_last[:, :, 0, :, None]) * state
                 + jnp.einsum('bhsd,bhsv->bhdv', kc * jnp.exp(g_last - G), ic))
        return state, o

    s0 = jnp.zeros((b, HGRN_HEADS, HGRN_DIM, HGRN_DIM), f32)
    _, o = lax.scan(step, s0, (chunks(q.astype(f32)), chunks(kk), chunks(i.astype(f32)), chunks(log_f)))
    return o.transpose(1, 0, 3, 2, 4).reshape(b, s, HGRN_HEADS, HGRN_DIM)


def _short_conv(u, w):
    kern = w[:, None, :].astype(u.dtype)
    return lax.conv_general_dilated(u, kern, (1,), [(CONV_K - 1, 0)],
                                    dimension_numbers=('NWC', 'WIO', 'NWC'),
                                    feature_group_count=u.shape[-1])


def setup_inputs(seed: int = 0) -> dict:
    key = jax.random.key(seed)
    ks = jax.random.split(key, 16)
    f32 = jnp.float32

    def nrm(k, shape, scale):
        return jax.random.normal(k, shape, f32) * scale

    def gain(k, shape):
        return 1.0 + 0.02 * jax.random.normal(k, shape, f32)

    return {
        'x': nrm(ks[0], (BATCH, SEQ, D_MODEL), 1.0),
        'rel_bias': nrm(ks[1], (NUM_BUCKETS, ATTN_HEADS), 0.5),
        'hgrn_lb': nrm(ks[2], (DEPTH, HGRN_W), 1.0),
        'ln_mix_g': gain(ks[3], (DEPTH, D_MODEL)),
        'w_in': nrm(ks[4], (DEPTH, D_MODEL, D_IN_PROJ), D_MODEL ** -0.5),
        'q_norm_g': gain(ks[5], (DEPTH, HEAD_DIM)),
        'k_norm_g': gain(ks[6], (DEPTH, HEAD_DIM)),
        'hgrn_norm_g': gain(ks[7], (DEPTH, HGRN_DIM)),
        'conv_w': nrm(ks[8], (DEPTH, CONV_K, CONV_W), CONV_K ** -0.5),
        'w_up_attn': nrm(ks[9], (DEPTH, ATTN_W, D_MODEL), ATTN_W ** -0.5),
        'w_up_hgrn': nrm(ks[10], (DEPTH, HGRN_W, D_MODEL), HGRN_W ** -0.5),
        'w_up_conv': nrm(ks[11], (DEPTH, CONV_W, D_MODEL), CONV_W ** -0.5),
        'w_out': nrm(ks[12], (DEPTH, D_MODEL, D_MODEL), D_MODEL ** -0.5),
        'ln_mlp_g': gain(ks[13], (DEPTH, D_MODEL)),
        'w_mlp_up': nrm(ks[14], (DEPTH, D_MODEL, D_FF), D_MODEL ** -0.5),
        'w_mlp_down': nrm(ks[15], (DEPTH, D_FF, D_MODEL), D_FF ** -0.5),
    }


def reference(x, rel_bias, hgrn_lb, ln_mix_g, w_in, q_norm_g, k_norm_g, hgrn_norm_g,
              conv_w, w_up_attn, w_up_hgrn, w_up_conv, w_out, ln_mlp_g, w_mlp_up, w_mlp_down):
    b, s, _ = x.shape
    lb_all = jnp.cumsum(jax.nn.softmax(hgrn_lb.astype(jnp.float32), axis=0), axis=0)
    lb_all = lb_all - lb_all[0]
    for l in range(DEPTH):
        h = _rmsnorm(x, ln_mix_g[l])
        proj = h @ w_in[l]
        (aq, ak, av, iq, ik, iw, hq, hf, hi, hg, cb, cc, ch, gates) = _split_cols(proj)
        q = _rmsnorm(aq.reshape(b, s, ATTN_HEADS, HEAD_DIM), q_norm_g[l])
        k = _rmsnorm(ak.reshape(b, s, ATTN_KV_GROUPS, HEAD_DIM), k_norm_g[l])
        v = av.reshape(b, s, ATTN_KV_GROUPS, HEAD_DIM)
        y_attn = _dsa_attention(q, k, v, iq.reshape(b, s, IDX_HEADS, IDX_DIM), ik, iw, rel_bias)
        o = _hgrn2(hq.reshape(b, s, HGRN_HEADS, HGRN_DIM), hf.reshape(b, s, HGRN_HEADS, HGRN_DIM),
                   hi.reshape(b, s, HGRN_HEADS, HGRN_DIM), lb_all[l])
        y_hgrn = (_rmsnorm(o, hgrn_norm_g[l]).astype(x.dtype)
                  * jax.nn.silu(hg.reshape(b, s, HGRN_HEADS, HGRN_DIM))).reshape(b, s, HGRN_W)
        y_conv = cb * _short_conv(cc * ch, conv_w[l])
        g = jax.nn.sigmoid(gates.reshape(b, s, N_BRANCH, D_MODEL))
        mixed = (g[:, :, 0] * (y_attn @ w_up_attn[l])
                 + g[:, :, 1] * (y_hgrn @ w_up_hgrn[l])
                 + g[:, :, 2] * (y_conv @ w_up_conv[l]))
        x = x + mixed @ w_out[l]
        h = _rmsnorm(x, ln_mlp_g[l])
        x = x + jnp.square(jax.nn.relu(h @ w_mlp_up[l])) @ w_mlp_down[l]
    return x
```

```python
import numpy as np
import concourse.bass as bass
import concourse.mybir as mybir
from concourse.bass_utils import run_bass_kernel_spmd

F32 = mybir.dt.float32
BF16 = mybir.dt.bfloat16
ALU = mybir.AluOpType
AF = mybir.ActivationFunctionType
AX = mybir.AxisListType

EPOCH = 30000


class Tok:
    def __init__(self, name, t=None):
        self.name = name
        self.t = t
        self.w = None
        self.r = {}
        self.dsem = None


class DSem:
    def __init__(self, sem):
        self.sem = sem
        self.val = 0


class Queue:
    def __init__(self, name):
        self.name = name
        self.ops = []
        self.count = 0
        self.waited = {}


class OpRec(list):
    pass


class Kern:
    def __init__(self, nc):
        self.nc = nc
        self.q = {n: Queue(n) for n in ("pe", "act", "dve", "pool", "sp")}
        self.out_deps = []
        self.nsem = 0
        self.cst = None
        self.cst_toks = []
        self.dsems = []

    def sb(self, name, shape, dtype):
        return Tok(name, self.nc.alloc_sbuf_tensor(name, list(shape), dtype))

    def ps(self, name, shape, dtype=F32):
        return Tok(name, self.nc.alloc_psum_tensor(name, list(shape), dtype))

    def tok(self, name):
        return Tok(name)

    def _newsem(self, name):
        self.nsem += 1
        return self.nc.alloc_semaphore(name)

    def _newdsem(self, name):
        ds = DSem(self._newsem(name))
        self.dsems.append(ds)
        return ds

    def _wait(self, Q, dep):
        if dep[0] == "q":
            _, qn, rec = dep
            seq = rec[2]
            if Q.waited.get(qn, 0) >= seq:
                return
            Q.waited[qn] = seq
            rec[3] = True
            Q.ops.append(["wait_q", qn, rec])
        else:
            _, ds, val = dep
            key = id(ds)
            if Q.waited.get(key, 0) >= val:
                return
            Q.waited[key] = val
            Q.ops.append(["wait_d", ds.sem, val])

    def _deps(self, qn, reads, writes):
        deps = []
        for b in reads:
            if b.w is not None:
                deps.append(b.w)
        for b in writes:
            if b.w is not None:
                deps.append(b.w)
            for d in b.r.values():
                if d[0] == "q" and d[1] == qn:
                    continue
                deps.append(d)
        out = []
        for d in deps:
            if d[0] == "q" and d[1] == qn and qn == "pe":
                continue
            out.append(d)
        return out

    def op(self, qn, fn, reads, writes):
        Q = self.q[qn]
        for d in self._deps(qn, reads, writes):
            self._wait(Q, d)
        Q.count += 1
        rec = OpRec(["op", fn, Q.count, False])
        Q.ops.append(rec)
        me = ("q", qn, rec)
        for b in writes:
            b.w = me
            b.r = {}
        for b in reads:
            if b not in writes:
                b.r[qn] = me
        return me

    def dma(self, qn, out, in_, reads, writes, sem_tok=None, final=False, const=False, **kw):
        Q = self.q[qn]
        if not const:
            for d in self._deps("dma", reads, writes):
                self._wait(Q, d)
        if const:
            if self.cst is None:
                self.cst = self._newdsem("s_cst")
            ds = self.cst
        else:
            if sem_tok is None:
                cand = [t for t in list(writes) + list(reads) if t.t is not None] or (list(writes) + list(reads))
                sem_tok = cand[0]
            if sem_tok.dsem is None:
                sem_tok.dsem = self._newdsem("d_" + sem_tok.name)
            ds = sem_tok.dsem
        ds.val += 16
        dep = ("d", ds, ds.val)
        Q.ops.append(["dma", out, in_, ds.sem, kw])
        for b in writes:
            b.w = dep
            b.r = {}
            if const:
                self.cst_toks.append(b)
        for b in reads:
            b.r[("d", id(ds))] = dep
        if final:
            self.out_deps.append(dep)
        return dep

    def const_done(self):
        if self.cst is None:
            return
        dep = ("d", self.cst, self.cst.val)
        for b in self.cst_toks:
            b.w = dep
        self.cst_toks = []

    def make_identity(self, ident):
        t = ident.t
        self.op("pool", lambda e: e.memset(t[:, :], 1.0), [], [ident])
        self.op("pool", lambda e: e.affine_select(out=t[:, :], in_=t[:, :], pattern=[[-1, 128]],
                                                  compare_op=ALU.is_equal, fill=0.0, base=0,
                                                  channel_multiplier=1), [ident], [ident])

    def finish(self):
        Q = self.q["sp"]
        for d in self.out_deps:
            self._wait(Q, d)
        for ds in self.dsems:
            if ds.val > 0:
                self._wait(Q, ("d", ds, ds.val))
        nc = self.nc
        self.nsig = {}
        for qn, Qx in self.q.items():
            k = 0
            sems = []
            for o in Qx.ops:
                if o[0] == "op" and o[3]:
                    e = k // EPOCH
                    while len(sems) <= e:
                        sems.append(self._newsem("s_%s_%d" % (qn, len(sems))))
                    o.append(sems[e])
                    o.append(k - e * EPOCH + 1)
                    k += 1
            self.nsig[qn] = k
        engs = {"pe": "tensor", "act": "scalar", "dve": "vector", "pool": "gpsimd", "sp": "sync"}
        with nc.Block() as block:
            for qn, attr in engs.items():
                ops = self.q[qn].ops

                def body(eng, ops=ops):
                    for o in ops:
                        if o[0] == "wait_q":
                            eng.wait_ge(o[2][4], o[2][5])
                        elif o[0] == "wait_d":
                            eng.wait_ge(o[1], o[2])
                        elif o[0] == "op":
                            ins = o[1](eng)
                            if o[3]:
                                ins.then_inc(o[4], 1)
                        else:
                            eng.dma_start(out=o[1], in_=o[2], **o[4]).then_inc(o[3], 16)

                getattr(block, attr)(body)

    def stats(self):
        return {qn: (Q.count, len(Q.ops), getattr(self, "nsig", {}).get(qn)) for qn, Q in self.q.items()}


D = 1024
NEG = -1.0e30
WIN_BLOCKS = [(0, 512), (512, 512), (1024, 68), (1092, 512), (1604, 512), (2116, 512), (2628, 512),
              (3140, 512), (3652, 512), (4164, 512)]
GATE0 = 4676
B_AQ, B_KVI, B_IKW, B_HQ, B_HF, B_HI, B_HG, B_CB, B_CC, B_CH = range(10)
B_GC, B_UC, B_GH, B_UH, B_GA, B_UA, B_WO, B_MLP = 10, 12, 13, 15, 16, 18, 19, 21
NBLK = 37
WNAMES = ["rel_bias", "hgrn_lb", "ln_mix_g", "w_in", "q_norm_g", "k_norm_g", "hgrn_norm_g", "conv_w",
          "w_up_attn", "w_up_hgrn", "w_up_conv", "w_out", "ln_mlp_g", "w_mlp_up", "w_mlp_down"]
WSHAPES = {"rel_bias": [32, 8], "hgrn_lb": [2, 512], "ln_mix_g": [2, 1024], "w_in": [2, 1024, 7748],
           "q_norm_g": [2, 64], "k_norm_g": [2, 64], "hgrn_norm_g": [2, 128], "conv_w": [2, 3, 512],
           "w_up_attn": [2, 512, 1024], "w_up_hgrn": [2, 512, 1024], "w_up_conv": [2, 512, 1024],
           "w_out": [2, 1024, 1024], "ln_mlp_g": [2, 1024], "w_mlp_up": [2, 1024, 4096],
           "w_mlp_down": [2, 4096, 1024]}


def t5_bucket_np(d):
    import math
    d = np.maximum(d, 0)
    df = np.maximum(d, 1).astype(np.float32)
    large = 16 + (np.log(df / np.float32(16)) / np.float32(math.log(128 / 16)) * np.float32(16)).astype(np.int32)
    large = np.minimum(large, 31)
    return np.where(d < 16, d, large)


def onehot_table():
    oh = np.zeros((32, 384), np.float32)
    for j in range(127, 384):
        oh[int(t5_bucket_np(np.array([j - 127]))[0]), j] = 1.0
    return oh


def dram_ap(handle_ap, offset, pattern):
    return bass.AP(tensor=handle_ap.tensor, offset=offset, ap=[list(p) for p in pattern])


(B_AQ, B_KVI, B_IKW, B_HI, B_HG, B_HQ, B_HF, B_GH, B_UH, B_CB, B_CC, B_CH, B_GC, B_UC, B_GA, B_UA,
 B_WO, B_MLP) = (0, 1, 2, 3, 4, 5, 6, 7, 9, 10, 11, 12, 13, 15, 16, 18, 19, 21)
WIN_COLS = {B_AQ: (0, 512), B_KVI: (512, 512), B_IKW: (1024, 128), B_HI: (2116, 512), B_HG: (2628, 512),
            B_HQ: (1092, 512), B_HF: (1604, 512), B_CB: (3140, 512), B_CC: (3652, 512), B_CH: (4164, 512),
            B_GH: (GATE0 + 1024, 512), B_GH + 1: (GATE0 + 1536, 512),
            B_GC: (GATE0 + 2048, 512), B_GC + 1: (GATE0 + 2560, 512),
            B_GA: (GATE0, 512), B_GA + 1: (GATE0 + 512, 512)}


import os as _os


def build_program(S, layers, NIT=22, dbg=None, upto=99):
    nc = bass.Bass("TRN2", target_bir_lowering=False)
    K = Kern(nc)
    NT = S // 512
    NB = S // 128
    NL = len(layers)
    op = K.op

    x_in = nc.dram_tensor("x", [S, D], F32, kind="ExternalInput").ap()
    out_d = nc.dram_tensor("out", [S, D], F32, kind="ExternalOutput").ap()
    W = {n: nc.dram_tensor(n, WSHAPES[n], F32, kind="ExternalInput").ap() for n in WNAMES}
    ohc_d = nc.dram_tensor("ohc", [32, 384], F32, kind="ExternalInput").ap()
    wscr = nc.dram_tensor("wscr", [NL * NBLK, 128, 4096], BF16, kind="Internal").ap()
    wtok = [K.tok("w%d" % i) for i in range(NL * NBLK)]
    xmid = [out_d for i in range(NL - 1)]
    xmid_tok = [K.tok("xmid") for _ in range(1)] * max(NL - 1, 1)
    toep = nc.dram_tensor("toep", [8, 128 * 384], F32, kind="Internal").ap()
    dbg_d = {}
    if dbg:
        for n, shp in dbg.items():
            dbg_d[n] = nc.dram_tensor("dbg_" + n, shp, F32, kind="ExternalOutput").ap()

    BIG = K.sb("BIG", [128, 8192], F32)
    BIGlo = BIG
    BIGhi = K.tok("BIGhi")
    SC = [BIGlo, BIGhi]
    xt = BIG.t[:, 0:4096].rearrange("p (a d) -> p a d", a=4)
    HI = BIG.t[:, 4096:8192]
    mixT = HI.rearrange("p (c t) -> p c t", c=8)

    def Ti(k, n=1):
        return HI[:, k * 512:(k + n) * 512]

    ARW = max(NB * 128, 8192)
    maskT = K.sb("maskT", [128, ARW], BF16)
    maskT3 = maskT.t[:, :].rearrange("p (b q) -> p b q", q=128)
    i64 = maskT.t[0:64, 0:4096].rearrange("p (c v) -> p c v", c=8)
    y64 = maskT.t[0:64, 4096:8192].rearrange("p (c v) -> p c v", c=8)
    mixTb = maskT.t[:, 0:4096].rearrange("p (c t) -> p c t", c=8)
    kT = K.sb("kT", [128, S], BF16)
    Vaug = K.sb("Vaug", [128, NB, 2, 65], BF16)
    kiT = K.sb("kiT", [64, S], BF16)
    ring = [K.sb("ring%d" % i, [128, 4096], BF16) for i in range(3)]
    hn = K.sb("hn", [128, 4, 1024], BF16)
    yTa = hn.t[:, 0:2, :].rearrange("p a (b t) -> p (a b) t", b=2)
    yTb = hn.t[:, 2:4, :].rearrange("p a (b t) -> p (a b) t", b=2)
    hT = K.sb("hT", [128, 8, 512], BF16)
    qT = K.sb("qT", [128, 4, 4, 128], BF16)
    uT = qT.t[:, :, :, :].rearrange("p a i q -> p a (i q)")
    qiT = K.sb("qiT", [64, 4, 4, 128], BF16)
    rt = [K.sb("rt%d" % i, [128, 512], F32) for i in range(2)]
    Eb = [K.sb("Eb%d" % i, [128, 512], BF16) for i in range(2)]
    Pb = [K.sb("Pb%d" % i, [128, 512], BF16) for i in range(2)]
    mrow = [K.sb("mrow%d" % i, [128, 512], BF16) for i in range(2)]
    qgb = K.sb("qgb", [128, 512], BF16)
    kgb = K.sb("kgb", [128, 512], BF16)
    TB = K.sb("TB", [128, 2, 8, 128], BF16)
    ident = K.sb("ident", [128, 128], BF16)
    tri64 = K.sb("tri64", [64, 64], F32)
    negtri = K.sb("negtri", [128, 128], F32)
    rmask = K.sb("rmask", [128, 512], F32)
    gq_b = K.sb("gq_b", [128, 2, 512], F32)
    gk_b = K.sb("gk_b", [128, 2, 128], F32)
    gn_b = K.sb("gn_b", [128, 2, 512], F32)
    lbraw = K.sb("lbraw", [128, 2, 4], F32)
    lbv = K.sb("lbv", [128, 2, 4], F32)
    oml = K.sb("oml", [128, 2, 4], F32)
    noml = K.sb("noml", [128, 2, 4], F32)
    cw = K.sb("cw", [128, 2, 4, 3], F32)
    gmix = K.sb("gmix", [128, 2, 8], F32)
    gmlp = K.sb("gmlp", [128, 2, 8], F32)
    rb = K.sb("rb", [32, 8], F32)
    rb31 = K.sb("rb31", [8, 1], F32)
    ssn = K.sb("ssn", [128, 4], F32)
    rsn = K.sb("rsn", [128, 4], F32)
    smq = K.sb("smq", [128, 8], F32)
    smk = K.sb("smk", [128, 2], F32)
    smo = K.sb("smo", [128, 8], F32)
    aabs = K.sb("aabs", [128, 4, 4], F32)
    asgn = K.sb("asgn", [128, 4, 4], F32)
    bis = K.sb("bis", [128, 8], F32)
    qh = K.sb("qh", [128, 4, 2, 64], BF16)
    kh = K.sb("kh", [128, 128], BF16)
    iqb = K.sb("iqb", [128, 256], BF16)
    ikb = K.sb("ikb", [128, 256], BF16)
    oev = K.sb("oev", [128, 8, 64], BF16)
    orc = K.sb("orc", [128, 8], F32)
    Shist = K.sb("Shist", [128, 9, 128], F32)
    Scar = K.sb("Scar", [128, 4, 128], F32)
    ubuf = K.sb("ubuf", [128, 4, 516], F32)
    kgT = K.sb("kgT", [64, 8, 128], BF16)
    ATb = K.sb("ATb", [64, 8, 64], BF16)
    stp = K.sb("stp", [128, 8, 128], BF16)
    hsm = K.sb("hsm", [128, 16], F32)

    psG = [K.ps("psG%d" % i, [128, 512], F32) for i in range(4)]
    psT = [K.ps("psT%d" % i, [128, 1024], BF16) for i in range(2)]
    psO = [K.ps("psO%d" % i, [128, 512], F32) for i in range(2)]
    cnt = {"g": 0, "t": 0, "e": 0, "r": 0}

    def pg():
        cnt["g"] += 1
        return psG[cnt["g"] % 4]

    def pt():
        cnt["t"] += 1
        return psT[cnt["t"] % 2]

    def nrt():
        cnt["r"] += 1
        return rt[cnt["r"] % 2]

    def alt(*names):
        cnt["e"] += 1
        return names[cnt["e"] % len(names)]

    def copy_any(q, out, in_, reads, writes):
        if q == "act":
            op("act", lambda e: e.copy(out=out, in_=in_), reads, writes)
        else:
            op(q, lambda e: e.tensor_copy(out=out, in_=in_), reads, writes)

    def dump(name, ap, tok):
        if dbg and name in dbg_d:
            K.dma("pool", dbg_d[name], ap, [tok], [], final=True, sem_tok=K.tok("dbg_" + name))

    def bc(ap, shape):
        return ap.to_broadcast(list(shape))

    def rstd_inplace(tok, ap, inv_n):
        op("dve", lambda e: e.tensor_scalar(out=ap, in0=ap, scalar1=inv_n, scalar2=1e-6, op0=ALU.mult,
                                            op1=ALU.add), [tok], [tok])
        op("act", lambda e: e.sqrt(out=ap, in_=ap), [tok], [tok])
        op("dve", lambda e: e.reciprocal(out=ap, in_=ap), [tok], [tok])

    K.make_identity(ident)
    op("pool", lambda e: e.memset(tri64.t[:, :], 1.0), [], [tri64])
    op("pool", lambda e: e.affine_select(out=tri64.t[:, :], in_=tri64.t[:, :], pattern=[[1, 64]],
                                         compare_op=ALU.is_ge, fill=0.0, base=0, channel_multiplier=-1),
       [tri64], [tri64])
    op("pool", lambda e: e.memset(negtri.t[:, :], 0.0), [], [negtri])
    op("pool", lambda e: e.affine_select(out=negtri.t[:, :], in_=negtri.t[:, :], pattern=[[-1, 128]],
                                         compare_op=ALU.is_ge, fill=NEG, base=0, channel_multiplier=1),
       [negtri], [negtri])
    op("pool", lambda e: e.memset(rmask.t[:, :], 1.0), [], [rmask])
    op("pool", lambda e: e.memset(rmask.t[:, :].rearrange("p (c t) -> p c t", t=64)[:, :, 0:1], 0.0),
       [rmask], [rmask])
    op("pool", lambda e: e.memset(Vaug.t[:, :, :, 64:65], 1.0), [], [Vaug])
    op("pool", lambda e: e.memset(kiT.t[:, :], 0.0), [], [kiT])
    op("pool", lambda e: e.memset(bis.t[:, :], 0.0), [], [bis])

    def cdma(out, in_, tok, **kw):
        K.dma("sp", out, in_, [], [tok], const=True, **kw)

    for li in range(2):
        cdma(gq_b.t[:, li, :].rearrange("p (h d) -> p h d", d=64),
             dram_ap(W["q_norm_g"], li * 64, [[0, 128], [0, 8], [1, 64]]), gq_b)
        cdma(gk_b.t[:, li, :].rearrange("p (h d) -> p h d", d=64),
             dram_ap(W["k_norm_g"], li * 64, [[0, 128], [0, 2], [1, 64]]), gk_b)
        cdma(gn_b.t[:, li, :].rearrange("p (h d) -> p h d", d=128),
             dram_ap(W["hgrn_norm_g"], li * 128, [[0, 128], [0, 4], [1, 128]]), gn_b)
    cdma(lbraw.t[:, :, :], dram_ap(W["hgrn_lb"], 0, [[1, 128], [512, 2], [128, 4]]), lbraw,
         allow_slow_non_contiguous=True)
    for li in range(2):
        for k in range(3):
            cdma(cw.t[:, li, :, k], dram_ap(W["conv_w"], li * 1536 + k * 512, [[1, 128], [128, 4]]), cw,
                 allow_slow_non_contiguous=True)
    cdma(gmix.t[:, :, :], dram_ap(W["ln_mix_g"], 0, [[1, 128], [1024, 2], [128, 8]]), gmix,
         allow_slow_non_contiguous=True)
    cdma(gmlp.t[:, :, :], dram_ap(W["ln_mlp_g"], 0, [[1, 128], [1024, 2], [128, 8]]), gmlp,
         allow_slow_non_contiguous=True)
    cdma(rb.t[:, :], W["rel_bias"], rb)
    ohc = rt[0]
    cdma(ohc.t[0:32, 0:384], ohc_d, ohc)
    cdma(rb31.t[:, :], dram_ap(W["rel_bias"], 31 * 8, [[1, 8], [1, 1]]), rb31)
    K.const_done()

    op("dve", lambda e: e.tensor_tensor(out=lbv.t[:, 1, :], in0=lbraw.t[:, 1, :], in1=lbraw.t[:, 0, :],
                                        op=ALU.subtract), [lbraw], [lbv])
    op("act", lambda e: e.activation(out=lbv.t[:, 1, :], in_=lbv.t[:, 1, :], func=AF.Sigmoid), [lbv], [lbv])
    op("dve", lambda e: e.memset(lbv.t[:, 0, :], 0.0), [lbv], [lbv])
    op("dve", lambda e: e.tensor_scalar(out=oml.t[:, :, :], in0=lbv.t[:, :, :], scalar1=-1.0, scalar2=1.0,
                                        op0=ALU.mult, op1=ALU.add), [lbv], [oml])
    op("dve", lambda e: e.tensor_scalar(out=noml.t[:, :, :], in0=oml.t[:, :, :], scalar1=-1.0, scalar2=None,
                                        op0=ALU.mult), [oml], [noml])

    pF = pg()
    Ft = rt[1]
    op("pe", lambda e: e.matmul(pF.t[0:8, 0:384], lhsT=rb.t[:, :], rhs=ohc.t[0:32, 0:384], start=True, stop=True),
       [rb, ohc], [pF])
    op("dve", lambda e: e.tensor_scalar(out=rb31.t[:, :], in0=rb31.t[:, :], scalar1=-1.0, scalar2=None,
                                        op0=ALU.mult), [rb31], [rb31])
    op("act", lambda e: e.activation(out=Ft.t[0:8, 0:384], in_=pF.t[0:8, 0:384], func=AF.Exp, bias=rb31.t[:, :]),
       [pF, rb31], [Ft])
    op("dve", lambda e: e.memset(Ft.t[0:8, 0:127], 0.0), [Ft], [Ft])
    toep_tok = K.tok("toep")
    K.dma("sp", toep.rearrange("h (r j) -> h r j", j=384),
          bc(Ft.t[0:8, 0:384].unsqueeze(1), [8, 128, 384]), [Ft], [toep_tok], sem_tok=Ft)
    tbs = BIG.t[:, 0:2048].rearrange("p (k h q) -> p k h q", k=2, h=8)
    for kind in range(2):
        for h in range(8):
            K.dma("sp", tbs[:, kind, h, :], dram_ap(toep, 127 + 128 * kind + h * 128 * 384, [[383, 128], [1, 128]]),
                  [toep_tok], [BIGlo])
    op("dve", lambda e: e.tensor_copy(out=TB.t[:, :, :, :], in_=tbs), [BIGlo], [TB])
    dump("TB", BIG.t[:, 0:2048], BIGlo)

    def wsrc(l, b):
        if b in WIN_COLS:
            c0, w = WIN_COLS[b]
            return W["w_in"][l][:, c0:c0 + w].rearrange("(c p) n -> p c n", p=128), 8, w, gmix.t[:, l, :]
        if b in (B_UH, B_UC, B_UA):
            nm = {B_UH: "w_up_hgrn", B_UC: "w_up_conv", B_UA: "w_up_attn"}[b]
            return W[nm][l].rearrange("(c p) n -> p c n", p=128), 4, 1024, None
        if b in (B_WO, B_WO + 1):
            h = b - B_WO
            return W["w_out"][l][:, h * 512:(h + 1) * 512].rearrange("(c p) n -> p c n", p=128), 8, 512, None
        g, isdn = divmod(b - B_MLP, 2)
        if not isdn:
            return (W["w_mlp_up"][l][:, g * 512:(g + 1) * 512].rearrange("(c p) n -> p c n", p=128), 8, 512,
                    gmlp.t[:, l, :])
        return W["w_mlp_down"][l][g * 512:(g + 1) * 512, :].rearrange("(c p) n -> p c n", p=128), 4, 1024, None

    stg = [(BIG.t[:, 0:4096], BIGlo), (BIG.t[:, 4096:8192], BIGhi)]
    for li, l in enumerate(layers):
        for b in [int(v) for v in _os.environ['PREP_B'].split(',')] if 'PREP_B' in _os.environ else range(NBLK if upto >= 1 else 0):
            src, nch, w, gain = wsrc(l, b)
            sap, stok = stg[b % 2]
            slot = ring[b % 3]
            n = nch * w
            K.dma("sp", sap[:, 0:n].rearrange("p (c n) -> p c n", c=nch), src, [], [stok])
            if gain is None:
                q = alt("dve", "act", "pool")
                copy_any(q, slot.t[:, 0:n], sap[:, 0:n], [stok], [slot])
            else:
                for c in range(nch):
                    q = alt("dve", "act")
                    o_ap = slot.t[:, c * w:(c + 1) * w]
                    i_ap = sap[:, c * w:(c + 1) * w]
                    g_ap = gain[:, c:c + 1]
                    if q == "dve":
                        op("dve", lambda e, o_ap=o_ap, i_ap=i_ap, g_ap=g_ap: e.tensor_scalar(
                            out=o_ap, in0=i_ap, scalar1=g_ap, scalar2=None, op0=ALU.mult), [stok, gmix, gmlp], [slot])
                    else:
                        op("act", lambda e, o_ap=o_ap, i_ap=i_ap, g_ap=g_ap: e.activation(
                            out=o_ap, in_=i_ap, func=AF.Copy, scale=g_ap), [stok, gmix, gmlp], [slot])
            K.dma("sp", wscr[li * NBLK + b][:, 0:n], slot.t[:, 0:n], [slot], [wtok[li * NBLK + b]])

    wstream = [li * NBLK + b for li in range(NL) for _ in range(NT) for b in range(NBLK)]
    wpos = {"issued": 0, "used": 0}

    def w_issue():
        n = wpos["issued"]
        if n >= len(wstream):
            return
        blk = wstream[n]
        nw = 1024 if blk % NBLK == B_IKW else 4096
        K.dma("sp", ring[n % 3].t[:, 0:nw], wscr[blk][:, 0:nw], [wtok[blk]], [ring[n % 3]])
        wpos["issued"] += 1

    def w_get(expect):
        n = wpos["used"]
        assert n < wpos["issued"], "weight block not issued"
        assert wstream[n] % NBLK == expect, (wstream[n] % NBLK, expect)
        wpos["used"] += 1
        return ring[n % 3]

    if upto >= 3:
        for _ in range(3):
            w_issue()

    def rms_to_hT():
        for a in range(4):
            op("act", lambda e, a=a: e.activation(out=hn.t[:, a, :], in_=xt[:, a, :], func=AF.Square,
                                                  accum_out=ssn.t[:, a:a + 1]), [BIGlo], [hn, ssn])
        op("dve", lambda e: e.tensor_scalar(out=rsn.t[:, :], in0=ssn.t[:, :], scalar1=1.0 / D, scalar2=1e-6,
                                            op0=ALU.mult, op1=ALU.add), [ssn], [rsn])
        op("act", lambda e: e.sqrt(out=rsn.t[:, :], in_=rsn.t[:, :]), [rsn], [rsn])
        op("dve", lambda e: e.reciprocal(out=rsn.t[:, :], in_=rsn.t[:, :]), [rsn], [rsn])
        for a in range(4):
            if a % 2 == 0:
                op("dve", lambda e, a=a: e.tensor_scalar(out=hn.t[:, a, :], in0=xt[:, a, :],
                                                         scalar1=rsn.t[:, a:a + 1], scalar2=None, op0=ALU.mult),
                   [BIGlo, rsn], [hn])
            else:
                op("act", lambda e, a=a: e.activation(out=hn.t[:, a, :], in_=xt[:, a, :], func=AF.Copy,
                                                      scale=rsn.t[:, a:a + 1]), [BIGlo, rsn], [hn])
        for c in range(8):
            p = pt()
            for a in range(4):
                op("pe", lambda e, a=a, c=c, p=p: e.transpose(out=p.t[:, a * 128:(a + 1) * 128],
                                                              in_=hn.t[:, a, c * 128:(c + 1) * 128],
                                                              identity=ident.t[:, :]), [hn, ident], [p])
            copy_any(alt("dve", "act"), hT.t[:, c, :], p.t[:, 0:512], [p], [hT])

    def mm_tm(p, a, slot, w, ncols=None, m0=None, m=128):
        ncols = w if ncols is None else ncols
        m0 = a * 128 if m0 is None else m0
        for c in range(8):
            op("pe", lambda e, c=c: e.matmul(p.t[0:m, 0:ncols], lhsT=hT.t[:, c, m0:m0 + m],
                                             rhs=slot.t[:, c * w:c * w + ncols], start=(c == 0), stop=(c == 7)),
               [hT, slot], [p])

    def mm_fm(p, slot, w, col0):
        for c in range(8):
            op("pe", lambda e, c=c: e.matmul(p.t[:, :], lhsT=slot.t[:, c * w + col0:c * w + col0 + 128],
                                             rhs=hT.t[:, c, :], start=(c == 0), stop=(c == 7)), [hT, slot], [p])

    def stage_qkv(l, j):
        slot = w_get(B_AQ)
        for a in range(4):
            p = pg()
            mm_tm(p, a, slot, 512)
            sq = nrt()
            op("act", lambda e, p=p, sq=sq: e.activation(out=sq.t[:, :], in_=p.t[:, :], func=AF.Square), [p], [sq])
            op("dve", lambda e, sq=sq: e.tensor_reduce(out=smq.t[:, :], in_=sq.t[:, :].rearrange("p (h d) -> p h d", d=64),
                                                       axis=AX.X, op=ALU.add), [sq], [smq])
            rstd_inplace(smq, smq.t[:, :], 1.0 / 64)
            op("dve", lambda e, p=p, sq=sq: e.tensor_tensor(
                out=sq.t[:, :].rearrange("p (h d) -> p h d", d=64), in0=p.t[:, :].rearrange("p (h d) -> p h d", d=64),
                in1=bc(smq.t[:, :].unsqueeze(2), [128, 8, 64]), op=ALU.mult), [p, smq], [sq])
            op("pool", lambda e, sq=sq: e.tensor_tensor(
                out=qh.t[:, :, :, :].rearrange("p i g d -> p g i d"),
                in0=sq.t[:, :].rearrange("p (g i d) -> p g i d", g=2, i=4),
                in1=gq_b.t[:, l, :].rearrange("p (g i d) -> p g i d", g=2, i=4), op=ALU.mult), [sq, gq_b], [qh])
            pT = pt()
            for i in range(4):
                op("pe", lambda e, i=i, pT=pT: e.transpose(out=pT.t[:, i * 128:(i + 1) * 128],
                                                           in_=qh.t[:, i, :, :].rearrange("p g d -> p (g d)"),
                                                           identity=ident.t[:, :]), [qh, ident], [pT])
            copy_any("act", qT.t[:, a, :, :], pT.t[:, 0:512].rearrange("p (i q) -> p i q", i=4), [pT], [qT])
        w_issue()
        if int(_os.environ.get("STOPQ", "9")) < 2:
            return
        slot = w_get(B_KVI)
        for a in range(4):
            blk = 4 * j + a
            p = pg()
            mm_tm(p, a, slot, 512)
            sq = nrt()
            op("act", lambda e, p=p, sq=sq: e.activation(out=sq.t[:, 0:128], in_=p.t[:, 0:128], func=AF.Square), [p], [sq])
            op("dve", lambda e, sq=sq: e.tensor_reduce(out=smk.t[:, :], in_=sq.t[:, 0:128].rearrange("p (h d) -> p h d", d=64),
                                                       axis=AX.X, op=ALU.add), [sq], [smk])
            rstd_inplace(smk, smk.t[:, :], 1.0 / 64)
            op("dve", lambda e, p=p, sq=sq: e.tensor_tensor(
                out=sq.t[:, 0:128].rearrange("p (h d) -> p h d", d=64),
                in0=p.t[:, 0:128].rearrange("p (h d) -> p h d", d=64),
                in1=bc(smk.t[:, :].unsqueeze(2), [128, 2, 64]), op=ALU.mult), [p, smk], [sq])
            op("pool", lambda e, sq=sq: e.tensor_tensor(out=kh.t[:, :], in0=sq.t[:, 0:128], in1=gk_b.t[:, l, :],
                                                        op=ALU.mult), [sq, gk_b], [kh])
            copy_any("act", Vaug.t[:, blk, :, 0:64], p.t[:, 128:256].rearrange("p (g d) -> p g d", g=2), [p], [Vaug])
            copy_any("dve", iqb.t[:, :], p.t[:, 256:512], [p], [iqb])
            pT = pt()
            op("pe", lambda e, pT=pT: e.transpose(out=pT.t[:, 0:128], in_=kh.t[:, :], identity=ident.t[:, :]),
               [kh, ident], [pT])
            copy_any("dve", kT.t[:, blk * 128:(blk + 1) * 128], pT.t[:, 0:128], [pT], [kT])
            pT2 = pt()
            for h in range(4):
                op("pe", lambda e, h=h, pT2=pT2: e.transpose(out=pT2.t[0:64, h * 128:(h + 1) * 128],
                                                             in_=iqb.t[:, h * 64:(h + 1) * 64],
                                                             identity=ident.t[:, :]), [iqb, ident], [pT2])
            copy_any("act", qiT.t[:, a, :, :], pT2.t[0:64, 0:512].rearrange("p (h q) -> p h q", h=4), [pT2], [qiT])
        w_issue()
        if int(_os.environ.get("STOPQ", "9")) < 3:
            return
        slot = w_get(B_IKW)
        pk = pg()
        for c in range(8):
            op("pe", lambda e, c=c, pk=pk: e.matmul(pk.t[0:64, :], lhsT=slot.t[:, c * 128:c * 128 + 64], rhs=hT.t[:, c, :],
                                                    start=(c == 0), stop=(c == 7)), [hT, slot], [pk])
        copy_any("act", kiT.t[:, j * 512:(j + 1) * 512], pk.t[0:64, :], [pk], [kiT])
        for a in range(4):
            p = pg()
            mm_tm(p, a, slot, 128)
            op("act", lambda e, p=p, a=a: e.activation(out=aabs.t[:, a, :], in_=p.t[:, 64:68], func=AF.Abs,
                                                       scale=1.0 / 16), [p], [aabs])
            op("dve", lambda e, p=p, a=a: e.tensor_scalar(out=asgn.t[:, a, :], in0=p.t[:, 64:68], scalar1=0.0, scalar2=2.0,
                                                          op0=ALU.is_ge, op1=ALU.mult), [p], [asgn])
            op("dve", lambda e, a=a: e.tensor_scalar(out=asgn.t[:, a, :], in0=asgn.t[:, a, :], scalar1=-1.0, scalar2=None,
                                                     op0=ALU.add), [asgn], [asgn])
        w_issue()

    def stage_attention(l, j, a):
        qb = 4 * j + a
        nch = qb // 4 + 1
        Wd = 512 * nch
        nvalid = 128 * (qb + 1)
        sc = BIG.t
        for ch in range(nch):
            scc = sc[:, ch * 512:(ch + 1) * 512]
            for h in range(4):
                p = pg()
                op("pe", lambda e, p=p, h=h, ch=ch: e.matmul(p.t[:, :], lhsT=qiT.t[:, a, h, :],
                                                             rhs=kiT.t[:, ch * 512:(ch + 1) * 512],
                                                             start=True, stop=True), [qiT, kiT], [p])
                r = nrt()
                op("act", lambda e, p=p, r=r, h=h: e.activation(out=r.t[:, :], in_=p.t[:, :], func=AF.Relu,
                                                                scale=aabs.t[:, a, h:h + 1]), [p, aabs], [r])
                if h == 0:
                    op("dve", lambda e, r=r, scc=scc: e.tensor_scalar(out=scc, in0=r.t[:, :], scalar1=asgn.t[:, a, 0:1],
                                                                      scalar2=None, op0=ALU.mult), [r, asgn], SC)
                else:
                    op("dve", lambda e, r=r, scc=scc, h=h: e.scalar_tensor_tensor(
                        out=scc, in0=r.t[:, :], scalar=asgn.t[:, a, h:h + 1], in1=scc, op0=ALU.mult, op1=ALU.add),
                       [r, asgn] + SC, SC)
        op("dve", lambda e: e.tensor_reduce(out=bis.t[:, 5:6], in_=sc[:, 0:nvalid], axis=AX.X, op=ALU.max,
                                            apply_absolute_value=True), SC, [bis])
        op("dve", lambda e: e.tensor_scalar(out=bis.t[:, 0:1], in0=bis.t[:, 5:6], scalar1=-1.0, scalar2=-1.0,
                                            op0=ALU.mult, op1=ALU.add), [bis], [bis])
        op("dve", lambda e: e.tensor_scalar(out=bis.t[:, 1:2], in0=bis.t[:, 5:6], scalar1=2.0, scalar2=2.0,
                                            op0=ALU.mult, op1=ALU.add), [bis], [bis])
        if nvalid < Wd:
            op("pool", lambda e: e.memset(sc[:, nvalid:Wd], NEG), SC + [bis], SC)
        op("pool", lambda e: e.tensor_tensor(out=sc[:, qb * 128:(qb + 1) * 128], in0=sc[:, qb * 128:(qb + 1) * 128],
                                             in1=negtri.t[:, :], op=ALU.add), SC + [negtri, bis], SC)
        for k in range(1, NIT + 1):
            f = 2.0 ** (-k)
            op("dve", lambda e, f=f: e.scalar_tensor_tensor(out=bis.t[:, 2:3], in0=bis.t[:, 1:2], scalar=f,
                                                            in1=bis.t[:, 0:1], op0=ALU.mult, op1=ALU.add), [bis], [bis])
            op("dve", lambda e: e.tensor_scalar(out=maskT.t[:, 0:Wd], in0=sc[:, 0:Wd], scalar1=bis.t[:, 2:3],
                                                scalar2=None, op0=ALU.is_ge, op1=ALU.add, accum_out=bis.t[:, 3:4]),
               SC + [bis], [maskT, bis])
            op("dve", lambda e: e.tensor_scalar(out=bis.t[:, 4:5], in0=bis.t[:, 3:4], scalar1=255.5,
                                                scalar2=bis.t[:, 1:2], op0=ALU.is_ge, op1=ALU.mult), [bis], [bis])
            op("dve", lambda e, f=f: e.scalar_tensor_tensor(out=bis.t[:, 0:1], in0=bis.t[:, 4:5], scalar=f,
                                                            in1=bis.t[:, 0:1], op0=ALU.mult, op1=ALU.add), [bis], [bis])
        for ch in range(nch):
            m = mrow[ch % 2]
            op("dve", lambda e, m=m, ch=ch: e.tensor_scalar(out=m.t[:, :], in0=sc[:, ch * 512:(ch + 1) * 512],
                                                            scalar1=bis.t[:, 0:1], scalar2=None, op0=ALU.is_ge),
               SC + [bis], [m])
            nk = min(4, qb + 1 - 4 * ch)
            pT = pt()
            for k4 in range(nk):
                op("pe", lambda e, m=m, k4=k4, pT=pT: e.transpose(out=pT.t[:, k4 * 128:(k4 + 1) * 128],
                                                                  in_=m.t[:, k4 * 128:(k4 + 1) * 128],
                                                                  identity=ident.t[:, :]), [m, ident], [pT])
            copy_any("act", maskT.t[:, ch * 512:ch * 512 + nk * 128], pT.t[:, 0:nk * 128], [pT], [maskT])
        for kb in range(qb + 1):
            kind = qb - kb
            for g in range(2):
                p = pg()
                op("pe", lambda e, p=p, g=g, kb=kb: e.matmul(
                    p.t[:, :], lhsT=kT.t[g * 64:(g + 1) * 64, kb * 128:(kb + 1) * 128],
                    rhs=qT.t[g * 64:(g + 1) * 64, a, :, :].rearrange("p i q -> p (i q)"), start=True, stop=True),
                   [kT, qT], [p])
                E = Eb[g]
                P = Pb[g]
                op("act", lambda e, p=p, E=E: e.activation(out=E.t[:, :], in_=p.t[:, :], func=AF.Exp, scale=0.125),
                   [p], [E])
                E3 = E.t[:, :].rearrange("p (h q) -> p h q", h=4)
                P3 = P.t[:, :].rearrange("p (h q) -> p h q", h=4)
                if kind <= 1:
                    op("pool", lambda e, E3=E3, g=g, kind=kind: e.tensor_tensor(
                        out=E3, in0=E3, in1=TB.t[:, kind, g * 4:(g + 1) * 4, :], op=ALU.mult), [E, TB], [E])
                op("dve", lambda e, E3=E3, P3=P3, kb=kb: e.tensor_tensor(
                    out=P3, in0=E3, in1=bc(maskT3[:, kb, :].unsqueeze(1), [128, 4, 128]), op=ALU.mult),
                   [E, maskT], [P])
                for h in range(4):
                    op("pe", lambda e, P=P, h=h, g=g, kb=kb: e.matmul(
                        psO[g].t[:, h * 65:(h + 1) * 65], lhsT=P.t[:, h * 128:(h + 1) * 128],
                        rhs=Vaug.t[:, kb, g, :], start=(kb == 0 and h == 0), stop=(kb == qb),
                        skip_group_check=True), [P, Vaug], [psO[g]])
        for g in range(2):
            o3 = psO[g].t[:, 0:260].rearrange("p (h e) -> p h e", e=65)
            op("dve", lambda e, g=g, o3=o3: e.reciprocal(out=orc.t[:, g * 4:(g + 1) * 4], in_=o3[:, :, 64]),
               [psO[g]], [orc])
            op("dve", lambda e, g=g, o3=o3: e.tensor_tensor(
                out=oev.t[:, g * 4:(g + 1) * 4, :], in0=o3[:, :, 0:64],
                in1=bc(orc.t[:, g * 4:(g + 1) * 4].unsqueeze(2), [128, 4, 64]), op=ALU.mult), [psO[g], orc], [oev])
        pT = pt()
        for fc in range(4):
            op("pe", lambda e, fc=fc, pT=pT: e.transpose(
                out=pT.t[:, fc * 128:(fc + 1) * 128], in_=oev.t[:, 2 * fc:2 * fc + 2, :].rearrange("p h d -> p (h d)"),
                identity=ident.t[:, :]), [oev, ident], [pT])
        copy_any("act", yTa[:, :, a * 128:(a + 1) * 128], pT.t[:, 0:512].rearrange("p (f q) -> p f q", f=4), [pT], [hn])

    def stage_hgrn(l, j):
        slot = w_get(B_HI)
        for c8 in range(8):
            p = pg()
            mm_tm(p, None, slot, 512, m0=c8 * 64, m=64)
            copy_any(alt("act", "dve"), i64[:, c8, :], p.t[0:64, :], [p], [maskT])
        w_issue()
        slot = w_get(B_HG)
        for c8 in range(8):
            p = pg()
            mm_tm(p, None, slot, 512, m0=c8 * 64, m=64)
            r = nrt()
            op("act", lambda e, p=p, r=r: e.activation(out=r.t[0:64, :], in_=p.t[0:64, :], func=AF.Silu), [p], [r])
            op("pool", lambda e, r=r, c8=c8: e.tensor_tensor(out=y64[:, c8, :], in0=r.t[0:64, :], in1=gn_b.t[0:64, l, :],
                                                             op=ALU.mult), [r, gn_b], [maskT])
        w_issue()
        slot = w_get(B_HQ)
        for hh in range(4):
            p = pg()
            mm_fm(p, slot, 512, hh * 128)
            copy_any("act", Ti(hh), p.t[:, :], [p], [BIGhi])
        w_issue()
        slot = w_get(B_HF)
        T4, T5, T6, T7 = Ti(4), Ti(5), Ti(6), Ti(7)
        Ut = Ti(4, 2).rearrange("p (c v) -> p c v", c=8)
        sqv = Ti(6, 2)
        for hh in range(4):
            p = pg()
            mm_fm(p, slot, 512, hh * 128)
            op("act", lambda e, p=p: e.activation(out=T4, in_=p.t[:, :], func=AF.Sigmoid), [p], [BIGhi])
            op("act", lambda e, hh=hh: e.activation(out=T5, in_=T4, func=AF.Ln, scale=oml.t[:, l, hh:hh + 1],
                                                    bias=lbv.t[:, l, hh:hh + 1]), [BIGhi, oml, lbv], [BIGhi])
            op("dve", lambda e, hh=hh: e.tensor_scalar(out=T4, in0=T4, scalar1=noml.t[:, l, hh:hh + 1],
                                                       scalar2=oml.t[:, l, hh:hh + 1], op0=ALU.mult, op1=ALU.add),
               [BIGhi, noml, oml], [BIGhi])
            op("dve", lambda e: e.tensor_tensor_scan(out=T6, data0=rmask.t[:, :], data1=T5, initial=0.0,
                                                     op0=ALU.mult, op1=ALU.add), [BIGhi, rmask], [BIGhi])
            G3 = T6.rearrange("p (c t) -> p c t", t=64)
            op("dve", lambda e, G3=G3: e.tensor_tensor(out=T5.rearrange("p (c t) -> p c t", t=64), in0=G3,
                                                       in1=bc(G3[:, :, 31:32], [128, 8, 64]), op=ALU.subtract),
               [BIGhi], [BIGhi])
            op("act", lambda e: e.activation(out=T7, in_=T5, func=AF.Exp), [BIGhi], [BIGhi])
            op("act", lambda e: e.activation(out=T5, in_=T5, func=AF.Exp, scale=-1.0), [BIGhi], [BIGhi])
            op("act", lambda e, G3=G3: e.activation(out=hsm.t[:, 0:8], in_=G3[:, :, 31], func=AF.Exp), [BIGhi], [hsm])
            op("act", lambda e, G3=G3: e.activation(out=hsm.t[:, 8:16], in_=G3[:, :, 63], func=AF.Exp), [BIGhi], [hsm])
            op("dve", lambda e, hh=hh: e.tensor_tensor(out=qgb.t[:, :], in0=Ti(hh), in1=T7, op=ALU.mult), [BIGhi], [qgb])
            op("pool", lambda e: e.tensor_tensor(out=kgb.t[:, :], in0=T4, in1=T5, op=ALU.mult), [BIGhi], [kgb])
            pA = pg()
            for c8 in range(8):
                op("pe", lambda e, c8=c8, pA=pA: e.matmul(pA.t[0:64, c8 * 64:(c8 + 1) * 64],
                                                          lhsT=kgb.t[:, c8 * 64:(c8 + 1) * 64],
                                                          rhs=qgb.t[:, c8 * 64:(c8 + 1) * 64], start=True, stop=True,
                                                          skip_group_check=True), [kgb, qgb], [pA])
            op("dve", lambda e, pA=pA: e.tensor_tensor(out=ATb.t[:, :, :],
                                                       in0=pA.t[0:64, :].rearrange("p (c t) -> p c t", t=64),
                                                       in1=bc(tri64.t[:, :].unsqueeze(1), [64, 8, 64]), op=ALU.mult),
               [pA, tri64], [ATb])
            pK = pt()
            for c8 in range(8):
                op("pe", lambda e, c8=c8, pK=pK: e.transpose(out=pK.t[0:64, c8 * 128:(c8 + 1) * 128],
                                                             in_=kgb.t[:, c8 * 64:(c8 + 1) * 64],
                                                             identity=ident.t[:, :]), [kgb, ident], [pK])
            copy_any("act", kgT.t[:, :, :], pK.t[0:64, :].rearrange("p (c d) -> p c d", c=8), [pK], [kgT])
            for c8 in range(8):
                op("pe", lambda e, c8=c8, hh=hh: e.matmul(psO[c8 // 4].t[:, (c8 % 4) * 128:(c8 % 4 + 1) * 128],
                                                          lhsT=kgT.t[:, c8, :], rhs=i64[:, c8, hh * 128:(hh + 1) * 128],
                                                          start=True, stop=True, skip_group_check=True),
                   [kgT, maskT], [psO[c8 // 4]])
            E63 = T7.rearrange("p (c t) -> p c t", t=64)
            for k in range(2):
                op("dve", lambda e, k=k, E63=E63: e.tensor_tensor(
                    out=Ut[:, 4 * k:4 * k + 4, :], in0=psO[k].t[:, :].rearrange("p (c v) -> p c v", c=4),
                    in1=bc(E63[:, 4 * k:4 * k + 4, 63:64], [128, 4, 128]), op=ALU.mult), [psO[k], BIGhi], [BIGhi])
            copy_any("pool", Shist.t[:, 0, :], Scar.t[:, hh, :], [Scar], [Shist])
            for c8 in range(8):
                op("dve", lambda e, c8=c8: e.scalar_tensor_tensor(out=Shist.t[:, c8 + 1, :], in0=Shist.t[:, c8, :],
                                                                  scalar=hsm.t[:, 8 + c8:9 + c8], in1=Ut[:, c8, :],
                                                                  op0=ALU.mult, op1=ALU.add), [Shist, hsm, BIGhi], [Shist])
            copy_any("pool", Scar.t[:, hh, :], Shist.t[:, 8, :], [Shist], [Scar])
            op("pool", lambda e: e.tensor_tensor(out=stp.t[:, :, :], in0=Shist.t[:, 0:8, :],
                                                 in1=bc(hsm.t[:, 0:8].unsqueeze(2), [128, 8, 128]), op=ALU.mult),
               [Shist, hsm], [stp])
            for c8 in range(8):
                reg = psO[c8 // 4].t[0:64, (c8 % 4) * 128:(c8 % 4 + 1) * 128]
                op("pe", lambda e, c8=c8, reg=reg, hh=hh: e.matmul(reg, lhsT=ATb.t[:, c8, :],
                                                                   rhs=i64[:, c8, hh * 128:(hh + 1) * 128],
                                                                   start=True, stop=False, skip_group_check=True),
                   [ATb, maskT], [psO[c8 // 4]])
                op("pe", lambda e, c8=c8, reg=reg: e.matmul(reg, lhsT=qgb.t[:, c8 * 64:(c8 + 1) * 64],
                                                            rhs=stp.t[:, c8, :], start=False, stop=True,
                                                            skip_group_check=True), [qgb, stp], [psO[c8 // 4]])
            for k in range(2):
                op("act", lambda e, k=k: e.activation(out=sqv[0:64, k * 512:(k + 1) * 512], in_=psO[k].t[0:64, :],
                                                      func=AF.Square), [psO[k]], [BIGhi])
            op("dve", lambda e: e.tensor_reduce(out=smo.t[0:64, :], in_=sqv[0:64, :].rearrange("p (c v) -> p c v", v=128),
                                                axis=AX.X, op=ALU.add), [BIGhi], [smo])
            rstd_inplace(smo, smo.t[0:64, :], 1.0 / 128)
            for k in range(2):
                op("dve", lambda e, k=k: e.tensor_tensor(
                    out=sqv[0:64, k * 512:(k + 1) * 512].rearrange("p (c v) -> p c v", c=4),
                    in0=psO[k].t[0:64, :].rearrange("p (c v) -> p c v", c=4),
                    in1=bc(smo.t[0:64, 4 * k:4 * k + 4].unsqueeze(2), [64, 4, 128]), op=ALU.mult),
                   [psO[k], smo, BIGhi], [BIGhi])
            op("pool", lambda e, hh=hh: e.tensor_tensor(out=y64[:, :, hh * 128:(hh + 1) * 128],
                                                        in0=sqv[0:64, :].rearrange("p (c v) -> p c v", v=128),
                                                        in1=y64[:, :, hh * 128:(hh + 1) * 128], op=ALU.mult),
               [BIGhi, maskT], [maskT])
        w_issue()
        for fc in range(4):
            pT = pt()
            for c8 in range(8):
                op("pe", lambda e, c8=c8, fc=fc, pT=pT: e.transpose(out=pT.t[:, c8 * 64:(c8 + 1) * 64],
                                                                    in_=y64[:, c8, fc * 128:(fc + 1) * 128],
                                                                    identity=ident.t[0:64, 0:64]), [maskT, ident], [pT])
            copy_any(alt("act", "dve"), yTb[:, fc, :], pT.t[:, 0:512], [pT], [hn])

    def stage_merge(l, bg, bu, yT, mode):
        gs = [w_get(bg), w_get(bg + 1)]
        us = w_get(bu)
        for mc in range(8):
            pgate = pg()
            mm_fm(pgate, gs[mc // 4], 512, (mc % 4) * 128)
            pU = pg()
            for fc in range(4):
                op("pe", lambda e, fc=fc, mc=mc, pU=pU: e.matmul(pU.t[:, :],
                                                                 lhsT=us.t[:, fc * 1024 + mc * 128:fc * 1024 + (mc + 1) * 128],
                                                                 rhs=yT[:, fc, :], start=(fc == 0), stop=(fc == 3)),
                   [us, hn], [pU])
            sg = nrt()
            op("act", lambda e, pgate=pgate, sg=sg: e.activation(out=sg.t[:, :], in_=pgate.t[:, :], func=AF.Sigmoid),
               [pgate], [sg])
            if mode == "first":
                op("dve", lambda e, sg=sg, pU=pU, mc=mc: e.tensor_tensor(out=mixT[:, mc, :], in0=pU.t[:, :], in1=sg.t[:, :],
                                                                         op=ALU.mult), [pU, sg], [BIGhi])
            else:
                op("dve", lambda e, sg=sg, pU=pU: e.tensor_tensor(out=sg.t[:, :], in0=pU.t[:, :], in1=sg.t[:, :],
                                                                  op=ALU.mult), [pU, sg], [sg])
                if mode == "acc":
                    op("pool", lambda e, sg=sg, mc=mc: e.tensor_tensor(out=mixT[:, mc, :], in0=mixT[:, mc, :],
                                                                       in1=sg.t[:, :], op=ALU.add), [sg, BIGhi], [BIGhi])
                else:
                    op("pool", lambda e, sg=sg, mc=mc: e.tensor_tensor(out=mixTb[:, mc, :], in0=mixT[:, mc, :],
                                                                       in1=sg.t[:, :], op=ALU.add), [sg, BIGhi], [maskT])
        w_issue()
        w_issue()
        w_issue()

    def stage_conv(l, j):
        sb_ = w_get(B_CB)
        sc_ = w_get(B_CC)
        sh_ = w_get(B_CH)
        for cc in range(4):
            pB, pC, pH = pg(), pg(), pg()
            mm_fm(pB, sb_, 512, cc * 128)
            mm_fm(pC, sc_, 512, cc * 128)
            mm_fm(pH, sh_, 512, cc * 128)
            r0 = nrt()
            copy_any("act", r0.t[:, :], pC.t[:, :], [pC], [r0])
            op("dve", lambda e, cc=cc, r0=r0, pH=pH: e.tensor_tensor(out=ubuf.t[:, cc, 2:514], in0=pH.t[:, :],
                                                                     in1=r0.t[:, :], op=ALU.mult), [pH, r0], [ubuf])
            r1 = nrt()
            op("dve", lambda e, cc=cc, r1=r1: e.tensor_scalar(out=r1.t[:, :], in0=ubuf.t[:, cc, 2:514],
                                                              scalar1=cw.t[:, l, cc, 2:3], scalar2=None, op0=ALU.mult),
               [ubuf, cw], [r1])
            op("dve", lambda e, cc=cc, r1=r1: e.scalar_tensor_tensor(out=r1.t[:, :], in0=ubuf.t[:, cc, 1:513],
                                                                     scalar=cw.t[:, l, cc, 1:2], in1=r1.t[:, :],
                                                                     op0=ALU.mult, op1=ALU.add), [ubuf, cw, r1], [r1])
            op("dve", lambda e, cc=cc, r1=r1: e.scalar_tensor_tensor(out=r1.t[:, :], in0=ubuf.t[:, cc, 0:512],
                                                                     scalar=cw.t[:, l, cc, 0:1], in1=r1.t[:, :],
                                                                     op0=ALU.mult, op1=ALU.add), [ubuf, cw, r1], [r1])
            op("dve", lambda e, cc=cc, r1=r1, pB=pB: e.tensor_tensor(out=yTb[:, cc, :], in0=pB.t[:, :], in1=r1.t[:, :],
                                                                     op=ALU.mult), [pB, r1], [hn])
            copy_any("pool", ubuf.t[:, cc, 0:2], ubuf.t[:, cc, 512:514], [ubuf], [ubuf])
        w_issue()
        w_issue()
        w_issue()

    def stage_wout(l):
        for half in range(2):
            slot = w_get(B_WO + half)
            for a in range(4):
                p = pg()
                for mc in range(8):
                    op("pe", lambda e, mc=mc, a=a, p=p, slot=slot: e.matmul(
                        p.t[:, :], lhsT=mixTb[:, mc, a * 128:(a + 1) * 128], rhs=slot.t[:, mc * 512:(mc + 1) * 512],
                        start=(mc == 0), stop=(mc == 7)), [maskT, slot], [p])
                xs = xt[:, a, half * 512:(half + 1) * 512]
                op("dve", lambda e, p=p, xs=xs: e.tensor_tensor(out=xs, in0=p.t[:, :], in1=xs, op=ALU.add),
                   [p, BIGlo], [BIGlo])
            w_issue()

    def stage_mlp(l):
        for g in range(8):
            su = w_get(B_MLP + 2 * g)
            for f4 in range(4):
                p = pg()
                mm_fm(p, su, 512, f4 * 128)
                rb_ = Eb[f4 % 2]
                op("act", lambda e, p=p, rb_=rb_: e.activation(out=rb_.t[:, :], in_=p.t[:, :], func=AF.Relu), [p], [rb_])
                op("pool", lambda e, rb_=rb_, f4=f4: e.tensor_tensor(out=uT[:, f4, :], in0=rb_.t[:, :], in1=rb_.t[:, :],
                                                                     op=ALU.mult), [rb_], [qT])
            w_issue()
            sd = w_get(B_MLP + 2 * g + 1)
            for a in range(4):
                for half in range(2):
                    p = pg()
                    for f4 in range(4):
                        op("pe", lambda e, f4=f4, a=a, half=half, p=p, sd=sd: e.matmul(
                            p.t[:, :], lhsT=uT[:, f4, a * 128:(a + 1) * 128],
                            rhs=sd.t[:, f4 * 1024 + half * 512:f4 * 1024 + (half + 1) * 512],
                            start=(f4 == 0), stop=(f4 == 3)), [qT, sd], [p])
                    xs = xt[:, a, half * 512:(half + 1) * 512]
                    op("dve", lambda e, p=p, xs=xs: e.tensor_tensor(out=xs, in0=p.t[:, :], in1=xs, op=ALU.add),
                       [p, BIGlo], [BIGlo])
            w_issue()

    for li, l in enumerate(layers):
        xsrc = x_in if li == 0 else xmid[li - 1]
        xsrc_tok = [] if li == 0 else [xmid_tok[li - 1]]
        xdst = out_d if li == NL - 1 else xmid[li]
        xdst_tok = [xmid_tok[0]] if NL > 1 else []
        op("pool", lambda e: e.memset(Scar.t[:, :, :], 0.0), [], [Scar])
        op("pool", lambda e: e.memset(ubuf.t[:, :, :], 0.0), [], [ubuf])
        for j in range(NT):
            xs_ap = xsrc[j * 512:(j + 1) * 512, :].rearrange("(a p) d -> p a d", p=128)
            xd_ap = xdst[j * 512:(j + 1) * 512, :].rearrange("(a p) d -> p a d", p=128)
            K.dma("sp", xt, xs_ap, xsrc_tok, [BIGlo])
            if upto >= 2:
                rms_to_hT()
                dump("hT", hT.t[:, :, :].rearrange("p c t -> p (c t)"), hT)
            if upto >= 3:
                stage_qkv(l, j)
                dump("qT", qT.t[:, :, :, :].rearrange("p a i q -> p (a i q)"), qT)
                dump("kT", kT.t[:, 0:512], kT)
            if upto >= 4:
                for a in range(4):
                    stage_attention(l, j, a)
                    if a == 0:
                        dump("sc", BIG.t[:, 0:512], BIGlo)
                        dump("bis", bis.t[:, :], bis)
                dump("yTa", hn.t[:, :, :].rearrange("p a t -> p (a t)"), hn)
            if upto >= 5:
                K.dma("sp", xt, xs_ap, xsrc_tok, [BIGlo])
                stage_hgrn(l, j)
                dump("yTb", hn.t[:, :, :].rearrange("p a t -> p (a t)"), hn)
            if upto >= 6:
                stage_merge(l, B_GH, B_UH, yTb, "first")
                stage_conv(l, j)
                stage_merge(l, B_GC, B_UC, yTb, "acc")
                stage_merge(l, B_GA, B_UA, yTa, "last")
                stage_wout(l)
                rms_to_hT()
                stage_mlp(l)
            K.dma("sp", xd_ap, xt, [BIGlo], xdst_tok, sem_tok=BIGlo, final=(li == NL - 1))
            if upto < 6:
                break
    K.finish()
    return nc, K


FUSED = True
SEQ_FULL = 8192


def _run(S, layers, xs, weights, NIT=22, dbg=None, trace=False, upto=99):
    nc, K = build_program(S, layers, NIT=NIT, dbg=dbg, upto=upto)
    ohc = onehot_table()
    in_maps = []
    for x in xs:
        m = {"x": np.ascontiguousarray(x, dtype=np.float32), "ohc": ohc}
        for n in WNAMES:
            m[n] = np.ascontiguousarray(weights[n], dtype=np.float32)
        in_maps.append(m)
    res = run_bass_kernel_spmd(nc, in_maps, core_ids=list(range(len(xs))), **({"trace": True} if trace else {}))
    return res


def kernel(**inputs):
    x = np.asarray(inputs["x"], dtype=np.float32)
    B, S, _ = x.shape
    weights = {n: np.asarray(inputs[n], dtype=np.float32) for n in WNAMES}
    xs = [x[b] for b in range(B)]
    if FUSED:
        res = _run(S, [0, 1], xs, weights)
        return np.stack([r["out"] for r in res.results], axis=0).astype(np.float32)
    for l in range(2):
        res = _run(S, [l], xs, weights)
        xs = [r["out"] for r in res.results]
    return np.stack(xs, axis=0).astype(np.float32)
```

```python
import numpy as np
import concourse.bass as bass
import concourse.mybir as mybir
from concourse.bass_utils import run_bass_kernel_spmd

F32 = mybir.dt.float32
BF16 = mybir.dt.bfloat16
ALU = mybir.AluOpType
AF = mybir.ActivationFunctionType
AX = mybir.AxisListType

EPOCH = 30000


class Tok:
    def __init__(self, name, t=None):
        self.name = name
        self.t = t
        self.w = None
        self.r = {}
        self.dsem = None


class DSem:
    def __init__(self, sem):
        self.sem = sem
        self.val = 0


class Queue:
    def __init__(self, name):
        self.name = name
        self.ops = []
        self.count = 0
        self.waited = {}


class OpRec(list):
    pass


class Kern:
    def __init__(self, nc):
        self.nc = nc
        self.q = {n: Queue(n) for n in ("pe", "act", "dve", "pool", "sp")}
        self.out_deps = []
        self.nsem = 0
        self.cst = None
        self.cst_toks = []
        self.dsems = []

    def sb(self, name, shape, dtype):
        return Tok(name, self.nc.alloc_sbuf_tensor(name, list(shape), dtype))

    def ps(self, name, shape, dtype=F32):
        return Tok(name, self.nc.alloc_psum_tensor(name, list(shape), dtype))

    def tok(self, name):
        return Tok(name)

    def _newsem(self, name):
        self.nsem += 1
        return self.nc.alloc_semaphore(name)

    def _newdsem(self, name):
        ds = DSem(self._newsem(name))
        self.dsems.append(ds)
        return ds

    def _wait(self, Q, dep):
        if dep[0] == "q":
            _, qn, rec = dep
            seq = rec[2]
            if Q.waited.get(qn, 0) >= seq:
                return
            Q.waited[qn] = seq
            rec[3] = True
            Q.ops.append(["wait_q", qn, rec])
        else:
            _, ds, val = dep
            key = id(ds)
            if Q.waited.get(key, 0) >= val:
                return
            Q.waited[key] = val
            Q.ops.append(["wait_d", ds.sem, val])

    def _deps(self, qn, reads, writes):
        deps = []
        for b in reads:
            if b.w is not None:
                deps.append(b.w)
        for b in writes:
            if b.w is not None:
                deps.append(b.w)
            for d in b.r.values():
                if d[0] == "q" and d[1] == qn:
                    continue
                deps.append(d)
        out = []
        for d in deps:
            if d[0] == "q" and d[1] == qn and qn == "pe":
                continue
            out.append(d)
        return out

    def op(self, qn, fn, reads, writes):
        Q = self.q[qn]
        for d in self._deps(qn, reads, writes):
            self._wait(Q, d)
        Q.count += 1
        rec = OpRec(["op", fn, Q.count, False])
        Q.ops.append(rec)
        me = ("q", qn, rec)
        for b in writes:
            b.w = me
            b.r = {}
        for b in reads:
            if b not in writes:
                b.r[qn] = me
        return me

    def dma(self, qn, out, in_, reads, writes, sem_tok=None, final=False, const=False, **kw):
        Q = self.q[qn]
        if not const:
            for d in self._deps("dma", reads, writes):
                self._wait(Q, d)
        if const:
            if self.cst is None:
                self.cst = self._newdsem("s_cst")
            ds = self.cst
        else:
            if sem_tok is None:
                cand = [t for t in list(writes) + list(reads) if t.t is not None] or (list(writes) + list(reads))
                sem_tok = cand[0]
            if sem_tok.dsem is None:
                sem_tok.dsem = self._newdsem("d_" + sem_tok.name)
            ds = sem_tok.dsem
        ds.val += 16
        dep = ("d", ds, ds.val)
        Q.ops.append(["dma", out, in_, ds.sem, kw])
        for b in writes:
            b.w = dep
            b.r = {}
            if const:
                self.cst_toks.append(b)
        for b in reads:
            b.r[("d", id(ds))] = dep
        if final:
            self.out_deps.append(dep)
        return dep

    def const_done(self):
        if self.cst is None:
            return
        dep = ("d", self.cst, self.cst.val)
        for b in self.cst_toks:
            b.w = dep
        self.cst_toks = []

    def make_identity(self, ident):
        t = ident.t
        self.op("pool", lambda e: e.memset(t[:, :], 1.0), [], [ident])
        self.op("pool", lambda e: e.affine_select(out=t[:, :], in_=t[:, :], pattern=[[-1, 128]],
                                                  compare_op=ALU.is_equal, fill=0.0, base=0,
                                                  channel_multiplier=1), [ident], [ident])

    def finish(self):
        Q = self.q["sp"]
        for d in self.out_deps:
            self._wait(Q, d)
        for ds in self.dsems:
            if ds.val > 0:
                self._wait(Q, ("d", ds, ds.val))
        nc = self.nc
        self.nsig = {}
        for qn, Qx in self.q.items():
            k = 0
            sems = []
            for o in Qx.ops:
                if o[0] == "op" and o[3]:
                    e = k // EPOCH
                    while len(sems) <= e:
                        sems.append(self._newsem("s_%s_%d" % (qn, len(sems))))
                    o.append(sems[e])
                    o.append(k - e * EPOCH + 1)
                    k += 1
            self.nsig[qn] = k
        engs = {"pe": "tensor", "act": "scalar", "dve": "vector", "pool": "gpsimd", "sp": "sync"}
        with nc.Block() as block:
            for qn, attr in engs.items():
                ops = self.q[qn].ops

                def body(eng, ops=ops):
                    for o in ops:
                        if o[0] == "wait_q":
                            eng.wait_ge(o[2][4], o[2][5])
                        elif o[0] == "wait_d":
                            eng.wait_ge(o[1], o[2])
                        elif o[0] == "op":
                            ins = o[1](eng)
                            if o[3]:
                                ins.then_inc(o[4], 1)
                        else:
                            eng.dma_start(out=o[1], in_=o[2], **o[4]).then_inc(o[3], 16)

                getattr(block, attr)(body)

    def stats(self):
        return {qn: (Q.count, len(Q.ops), getattr(self, "nsig", {}).get(qn)) for qn, Q in self.q.items()}


D = 1024
NEG = -1.0e30
WIN_BLOCKS = [(0, 512), (512, 512), (1024, 68), (1092, 512), (1604, 512), (2116, 512), (2628, 512),
              (3140, 512), (3652, 512), (4164, 512)]
GATE0 = 4676
B_AQ, B_KVI, B_IKW, B_HQ, B_HF, B_HI, B_HG, B_CB, B_CC, B_CH = range(10)
B_GC, B_UC, B_GH, B_UH, B_GA, B_UA, B_WO, B_MLP = 10, 12, 13, 15, 16, 18, 19, 21
NBLK = 37
WNAMES = ["rel_bias", "hgrn_lb", "ln_mix_g", "w_in", "q_norm_g", "k_norm_g", "hgrn_norm_g", "conv_w",
          "w_up_attn", "w_up_hgrn", "w_up_conv", "w_out", "ln_mlp_g", "w_mlp_up", "w_mlp_down"]
WSHAPES = {"rel_bias": [32, 8], "hgrn_lb": [2, 512], "ln_mix_g": [2, 1024], "w_in": [2, 1024, 7748],
           "q_norm_g": [2, 64], "k_norm_g": [2, 64], "hgrn_norm_g": [2, 128], "conv_w": [2, 3, 512],
           "w_up_attn": [2, 512, 1024], "w_up_hgrn": [2, 512, 1024], "w_up_conv": [2, 512, 1024],
           "w_out": [2, 1024, 1024], "ln_mlp_g": [2, 1024], "w_mlp_up": [2, 1024, 4096],
           "w_mlp_down": [2, 4096, 1024]}


def t5_bucket_np(d):
    import math
    d = np.maximum(d, 0)
    df = np.maximum(d, 1).astype(np.float32)
    large = 16 + (np.log(df / np.float32(16)) / np.float32(math.log(128 / 16)) * np.float32(16)).astype(np.int32)
    large = np.minimum(large, 31)
    return np.where(d < 16, d, large)


def onehot_table():
    oh = np.zeros((32, 384), np.float32)
    for j in range(127, 384):
        oh[int(t5_bucket_np(np.array([j - 127]))[0]), j] = 1.0
    return oh


def dram_ap(handle_ap, offset, pattern):
    return bass.AP(tensor=handle_ap.tensor, offset=offset, ap=[list(p) for p in pattern])


(B_AQ, B_KVI, B_IKW, B_HI, B_HG, B_HQ, B_HF, B_GH, B_UH, B_CB, B_CC, B_CH, B_GC, B_UC, B_GA, B_UA,
 B_WO, B_MLP) = (0, 1, 2, 3, 4, 5, 6, 7, 9, 10, 11, 12, 13, 15, 16, 18, 19, 21)
WIN_COLS = {B_AQ: (0, 512), B_KVI: (512, 512), B_IKW: (1024, 128), B_HI: (2116, 512), B_HG: (2628, 512),
            B_HQ: (1092, 512), B_HF: (1604, 512), B_CB: (3140, 512), B_CC: (3652, 512), B_CH: (4164, 512),
            B_GH: (GATE0 + 1024, 512), B_GH + 1: (GATE0 + 1536, 512),
            B_GC: (GATE0 + 2048, 512), B_GC + 1: (GATE0 + 2560, 512),
            B_GA: (GATE0, 512), B_GA + 1: (GATE0 + 512, 512)}


import os as _os


def build_program(S, layers, NIT=17, dbg=None, upto=99):
    nc = bass.Bass("TRN2", target_bir_lowering=False)
    K = Kern(nc)
    NT = S // 512
    NB = S // 128
    NL = len(layers)
    op = K.op

    x_in = nc.dram_tensor("x", [S, D], F32, kind="ExternalInput").ap()
    out_d = nc.dram_tensor("out", [S, D], F32, kind="ExternalOutput").ap()
    W = {n: nc.dram_tensor(n, WSHAPES[n], F32, kind="ExternalInput").ap() for n in WNAMES}
    ohc_d = nc.dram_tensor("ohc", [32, 384], F32, kind="ExternalInput").ap()
    wscr = nc.dram_tensor("wscr", [NL * NBLK, 128, 4096], BF16, kind="Internal").ap()
    wtok = [K.tok("w%d" % i) for i in range(NL * NBLK)]
    xmid = [out_d for i in range(NL - 1)]
    xmid_tok = [K.tok("xmid") for _ in range(1)] * max(NL - 1, 1)
    toep = nc.dram_tensor("toep", [8, 128 * 384], F32, kind="Internal").ap()
    dbg_d = {}
    if dbg:
        for n, shp in dbg.items():
            dbg_d[n] = nc.dram_tensor("dbg_" + n, shp, F32, kind="ExternalOutput").ap()

    BIG = K.sb("BIG", [128, 8192], F32)
    BIGlo = BIG
    BIGhi = K.tok("BIGhi")
    SC = [BIGlo, BIGhi]
    xt = BIG.t[:, 0:4096].rearrange("p (a d) -> p a d", a=4)
    HI = BIG.t[:, 4096:8192]
    mixT = HI.rearrange("p (c t) -> p c t", c=8)

    def Ti(k, n=1):
        return HI[:, k * 512:(k + n) * 512]

    ARW = max(NB * 128, 8192)
    maskT = K.sb("maskT", [128, ARW], BF16)
    maskT3 = maskT.t[:, :].rearrange("p (b q) -> p b q", q=128)
    i64 = maskT.t[0:64, 0:4096].rearrange("p (c v) -> p c v", c=8)
    y64 = maskT.t[0:64, 4096:8192].rearrange("p (c v) -> p c v", c=8)
    mixTb = maskT.t[:, 0:4096].rearrange("p (c t) -> p c t", c=8)
    kT = K.sb("kT", [128, S], BF16)
    Vaug = K.sb("Vaug", [128, NB, 2, 65], BF16)
    kiT = K.sb("kiT", [64, S], BF16)
    ring = [K.sb("ring%d" % i, [128, 4096], BF16) for i in range(3)]
    hn = K.sb("hn", [128, 4, 1024], BF16)
    yTa = hn.t[:, 0:2, :].rearrange("p a (b t) -> p (a b) t", b=2)
    yTb = hn.t[:, 2:4, :].rearrange("p a (b t) -> p (a b) t", b=2)
    hT = K.sb("hT", [128, 8, 512], BF16)
    qT = K.sb("qT", [128, 4, 4, 128], BF16)
    uT = qT.t[:, :, :, :].rearrange("p a i q -> p a (i q)")
    qiT = K.sb("qiT", [64, 4, 4, 128], BF16)
    rt = [K.sb("rt%d" % i, [128, 512], F32) for i in range(2)]
    Eb = [K.sb("Eb%d" % i, [128, 512], BF16) for i in range(2)]
    Pb = [K.sb("Pb%d" % i, [128, 512], BF16) for i in range(2)]
    mrow = [K.sb("mrow%d" % i, [128, 512], BF16) for i in range(2)]
    qgb = K.sb("qgb", [128, 512], BF16)
    kgb = K.sb("kgb", [128, 512], BF16)
    TB = K.sb("TB", [128, 2, 8, 128], BF16)
    ident = K.sb("ident", [128, 128], BF16)
    tri64 = K.sb("tri64", [64, 64], F32)
    negtri = K.sb("negtri", [128, 128], F32)
    rmask = K.sb("rmask", [128, 512], F32)
    gq_b = K.sb("gq_b", [128, 2, 512], F32)
    gk_b = K.sb("gk_b", [128, 2, 128], F32)
    gn_b = K.sb("gn_b", [128, 2, 512], F32)
    lbraw = K.sb("lbraw", [128, 2, 4], F32)
    lbv = K.sb("lbv", [128, 2, 4], F32)
    oml = K.sb("oml", [128, 2, 4], F32)
    noml = K.sb("noml", [128, 2, 4], F32)
    cw = K.sb("cw", [128, 2, 4, 3], F32)
    gmix = K.sb("gmix", [128, 2, 8], F32)
    gmlp = K.sb("gmlp", [128, 2, 8], F32)
    rb = K.sb("rb", [32, 8], F32)
    rb31 = K.sb("rb31", [8, 1], F32)
    ssn = K.sb("ssn", [128, 4], F32)
    rsn = K.sb("rsn", [128, 4], F32)
    smq = K.sb("smq", [128, 8], F32)
    smk = K.sb("smk", [128, 2], F32)
    smo = K.sb("smo", [128, 8], F32)
    aabs = K.sb("aabs", [128, 4, 4], F32)
    asgn = K.sb("asgn", [128, 4, 4], F32)
    bis = K.sb("bis", [128, 8], F32)
    bisA = K.sb("bisA", [128, 2], F32)
    nbv = K.sb("nbv", [128, 2], F32)
    junkA = K.tok("junkA")
    qh = K.sb("qh", [128, 4, 2, 64], BF16)
    kh = K.sb("kh", [128, 128], BF16)
    iqb = K.sb("iqb", [128, 256], BF16)
    ikb = K.sb("ikb", [128, 256], BF16)
    oev = K.sb("oev", [128, 8, 64], BF16)
    orc = K.sb("orc", [128, 8], F32)
    Shist = K.sb("Shist", [128, 9, 128], F32)
    Scar = K.sb("Scar", [128, 4, 128], F32)
    ubuf = K.sb("ubuf", [128, 4, 516], F32)
    kgT = K.sb("kgT", [64, 8, 128], BF16)
    ATb = K.sb("ATb", [64, 8, 64], BF16)
    stp = K.sb("stp", [128, 8, 128], BF16)
    hsm = K.sb("hsm", [128, 16], F32)

    psG = [K.ps("psG%d" % i, [128, 512], F32) for i in range(4)]
    psT = [K.ps("psT%d" % i, [128, 1024], BF16) for i in range(2)]
    psO = [K.ps("psO%d" % i, [128, 512], F32) for i in range(2)]
    cnt = {"g": 0, "t": 0, "e": 0, "r": 0}

    def pg():
        cnt["g"] += 1
        return psG[cnt["g"] % 4]

    def pt():
        cnt["t"] += 1
        return psT[cnt["t"] % 2]

    def nrt():
        cnt["r"] += 1
        return rt[cnt["r"] % 2]

    def alt(*names):
        cnt["e"] += 1
        return names[cnt["e"] % len(names)]

    def copy_any(q, out, in_, reads, writes):
        if q == "act":
            op("act", lambda e: e.copy(out=out, in_=in_), reads, writes)
        else:
            op(q, lambda e: e.tensor_copy(out=out, in_=in_), reads, writes)

    def dump(name, ap, tok):
        if dbg and name in dbg_d:
            K.dma("pool", dbg_d[name], ap, [tok], [], final=True, sem_tok=K.tok("dbg_" + name))

    def bc(ap, shape):
        return ap.to_broadcast(list(shape))

    def rstd_inplace(tok, ap, inv_n):
        op("dve", lambda e: e.tensor_scalar(out=ap, in0=ap, scalar1=inv_n, scalar2=1e-6, op0=ALU.mult,
                                            op1=ALU.add), [tok], [tok])
        op("act", lambda e: e.sqrt(out=ap, in_=ap), [tok], [tok])
        op("dve", lambda e: e.reciprocal(out=ap, in_=ap), [tok], [tok])

    K.make_identity(ident)
    op("pool", lambda e: e.memset(tri64.t[:, :], 1.0), [], [tri64])
    op("pool", lambda e: e.affine_select(out=tri64.t[:, :], in_=tri64.t[:, :], pattern=[[1, 64]],
                                         compare_op=ALU.is_ge, fill=0.0, base=0, channel_multiplier=-1),
       [tri64], [tri64])
    op("pool", lambda e: e.memset(negtri.t[:, :], 0.0), [], [negtri])
    op("pool", lambda e: e.affine_select(out=negtri.t[:, :], in_=negtri.t[:, :], pattern=[[-1, 128]],
                                         compare_op=ALU.is_ge, fill=NEG, base=0, channel_multiplier=1),
       [negtri], [negtri])
    op("pool", lambda e: e.memset(rmask.t[:, :], 1.0), [], [rmask])
    op("pool", lambda e: e.memset(rmask.t[:, :].rearrange("p (c t) -> p c t", t=64)[:, :, 0:1], 0.0),
       [rmask], [rmask])
    op("pool", lambda e: e.memset(Vaug.t[:, :, :, 64:65], 1.0), [], [Vaug])
    op("pool", lambda e: e.memset(kiT.t[:, :], 0.0), [], [kiT])
    op("pool", lambda e: e.memset(bis.t[:, :], 0.0), [], [bis])

    def cdma(out, in_, tok, **kw):
        K.dma("sp", out, in_, [], [tok], const=True, **kw)

    for li in range(2):
        cdma(gq_b.t[:, li, :].rearrange("p (h d) -> p h d", d=64),
             dram_ap(W["q_norm_g"], li * 64, [[0, 128], [0, 8], [1, 64]]), gq_b)
        cdma(gk_b.t[:, li, :].rearrange("p (h d) -> p h d", d=64),
             dram_ap(W["k_norm_g"], li * 64, [[0, 128], [0, 2], [1, 64]]), gk_b)
        cdma(gn_b.t[:, li, :].rearrange("p (h d) -> p h d", d=128),
             dram_ap(W["hgrn_norm_g"], li * 128, [[0, 128], [0, 4], [1, 128]]), gn_b)
    cdma(lbraw.t[:, :, :], dram_ap(W["hgrn_lb"], 0, [[1, 128], [512, 2], [128, 4]]), lbraw,
         allow_slow_non_contiguous=True)
    for li in range(2):
        for k in range(3):
            cdma(cw.t[:, li, :, k], dram_ap(W["conv_w"], li * 1536 + k * 512, [[1, 128], [128, 4]]), cw,
                 allow_slow_non_contiguous=True)
    cdma(gmix.t[:, :, :], dram_ap(W["ln_mix_g"], 0, [[1, 128], [1024, 2], [128, 8]]), gmix,
         allow_slow_non_contiguous=True)
    cdma(gmlp.t[:, :, :], dram_ap(W["ln_mlp_g"], 0, [[1, 128], [1024, 2], [128, 8]]), gmlp,
         allow_slow_non_contiguous=True)
    cdma(rb.t[:, :], W["rel_bias"], rb)
    ohc = rt[0]
    cdma(ohc.t[0:32, 0:384], ohc_d, ohc)
    cdma(rb31.t[:, :], dram_ap(W["rel_bias"], 31 * 8, [[1, 8], [1, 1]]), rb31)
    K.const_done()

    op("dve", lambda e: e.tensor_tensor(out=lbv.t[:, 1, :], in0=lbraw.t[:, 1, :], in1=lbraw.t[:, 0, :],
                                        op=ALU.subtract), [lbraw], [lbv])
    op("act", lambda e: e.activation(out=lbv.t[:, 1, :], in_=lbv.t[:, 1, :], func=AF.Sigmoid), [lbv], [lbv])
    op("dve", lambda e: e.memset(lbv.t[:, 0, :], 0.0), [lbv], [lbv])
    op("dve", lambda e: e.tensor_scalar(out=oml.t[:, :, :], in0=lbv.t[:, :, :], scalar1=-1.0, scalar2=1.0,
                                        op0=ALU.mult, op1=ALU.add), [lbv], [oml])
    op("dve", lambda e: e.tensor_scalar(out=noml.t[:, :, :], in0=oml.t[:, :, :], scalar1=-1.0, scalar2=None,
                                        op0=ALU.mult), [oml], [noml])

    pF = pg()
    Ft = rt[1]
    op("pe", lambda e: e.matmul(pF.t[0:8, 0:384], lhsT=rb.t[:, :], rhs=ohc.t[0:32, 0:384], start=True, stop=True),
       [rb, ohc], [pF])
    op("dve", lambda e: e.tensor_scalar(out=rb31.t[:, :], in0=rb31.t[:, :], scalar1=-1.0, scalar2=None,
                                        op0=ALU.mult), [rb31], [rb31])
    op("act", lambda e: e.activation(out=Ft.t[0:8, 0:384], in_=pF.t[0:8, 0:384], func=AF.Exp, bias=rb31.t[:, :]),
       [pF, rb31], [Ft])
    op("dve", lambda e: e.memset(Ft.t[0:8, 0:127], 0.0), [Ft], [Ft])
    toep_tok = K.tok("toep")
    K.dma("sp", toep.rearrange("h (r j) -> h r j", j=384),
          bc(Ft.t[0:8, 0:384].unsqueeze(1), [8, 128, 384]), [Ft], [toep_tok], sem_tok=Ft)
    tbs = BIG.t[:, 0:2048].rearrange("p (k h q) -> p k h q", k=2, h=8)
    for kind in range(2):
        for h in range(8):
            K.dma("sp", tbs[:, kind, h, :], dram_ap(toep, 127 + 128 * kind + h * 128 * 384, [[383, 128], [1, 128]]),
                  [toep_tok], [BIGlo])
    op("dve", lambda e: e.tensor_copy(out=TB.t[:, :, :, :], in_=tbs), [BIGlo], [TB])
    dump("TB", BIG.t[:, 0:2048], BIGlo)

    def wsrc(l, b):
        if b in WIN_COLS:
            c0, w = WIN_COLS[b]
            return W["w_in"][l][:, c0:c0 + w].rearrange("(c p) n -> p c n", p=128), 8, w, gmix.t[:, l, :]
        if b in (B_UH, B_UC, B_UA):
            nm = {B_UH: "w_up_hgrn", B_UC: "w_up_conv", B_UA: "w_up_attn"}[b]
            return W[nm][l].rearrange("(c p) n -> p c n", p=128), 4, 1024, None
        if b in (B_WO, B_WO + 1):
            h = b - B_WO
            return W["w_out"][l][:, h * 512:(h + 1) * 512].rearrange("(c p) n -> p c n", p=128), 8, 512, None
        g, isdn = divmod(b - B_MLP, 2)
        if not isdn:
            return (W["w_mlp_up"][l][:, g * 512:(g + 1) * 512].rearrange("(c p) n -> p c n", p=128), 8, 512,
                    gmlp.t[:, l, :])
        return W["w_mlp_down"][l][g * 512:(g + 1) * 512, :].rearrange("(c p) n -> p c n", p=128), 4, 1024, None

    stg = [(BIG.t[:, 0:4096], BIGlo), (BIG.t[:, 4096:8192], BIGhi)]
    for li, l in enumerate(layers):
        for b in [int(v) for v in _os.environ['PREP_B'].split(',')] if 'PREP_B' in _os.environ else range(NBLK if upto >= 1 else 0):
            src, nch, w, gain = wsrc(l, b)
            sap, stok = stg[b % 2]
            slot = ring[b % 3]
            n = nch * w
            K.dma("sp", sap[:, 0:n].rearrange("p (c n) -> p c n", c=nch), src, [], [stok])
            if gain is None:
                q = alt("dve", "act", "pool")
                copy_any(q, slot.t[:, 0:n], sap[:, 0:n], [stok], [slot])
            else:
                for c in range(nch):
                    q = alt("dve", "act")
                    o_ap = slot.t[:, c * w:(c + 1) * w]
                    i_ap = sap[:, c * w:(c + 1) * w]
                    g_ap = gain[:, c:c + 1]
                    if q == "dve":
                        op("dve", lambda e, o_ap=o_ap, i_ap=i_ap, g_ap=g_ap: e.tensor_scalar(
                            out=o_ap, in0=i_ap, scalar1=g_ap, scalar2=None, op0=ALU.mult), [stok, gmix, gmlp], [slot])
                    else:
                        op("act", lambda e, o_ap=o_ap, i_ap=i_ap, g_ap=g_ap: e.activation(
                            out=o_ap, in_=i_ap, func=AF.Copy, scale=g_ap), [stok, gmix, gmlp], [slot])
            K.dma("sp", wscr[li * NBLK + b][:, 0:n], slot.t[:, 0:n], [slot], [wtok[li * NBLK + b]])

    wstream = [li * NBLK + b for li in range(NL) for _ in range(NT) for b in range(NBLK)]
    wpos = {"issued": 0, "used": 0}

    def w_issue():
        n = wpos["issued"]
        if n >= len(wstream):
            return
        blk = wstream[n]
        nw = 1024 if blk % NBLK == B_IKW else 4096
        K.dma("sp", ring[n % 3].t[:, 0:nw], wscr[blk][:, 0:nw], [wtok[blk]], [ring[n % 3]])
        wpos["issued"] += 1

    def w_get(expect):
        n = wpos["used"]
        assert n < wpos["issued"], "weight block not issued"
        assert wstream[n] % NBLK == expect, (wstream[n] % NBLK, expect)
        wpos["used"] += 1
        return ring[n % 3]

    if upto >= 3:
        for _ in range(3):
            w_issue()

    def rms_to_hT():
        for a in range(4):
            op("act", lambda e, a=a: e.activation(out=hn.t[:, a, :], in_=xt[:, a, :], func=AF.Square,
                                                  accum_out=ssn.t[:, a:a + 1]), [BIGlo], [hn, ssn])
        op("dve", lambda e: e.tensor_scalar(out=rsn.t[:, :], in0=ssn.t[:, :], scalar1=1.0 / D, scalar2=1e-6,
                                            op0=ALU.mult, op1=ALU.add), [ssn], [rsn])
        op("act", lambda e: e.sqrt(out=rsn.t[:, :], in_=rsn.t[:, :]), [rsn], [rsn])
        op("dve", lambda e: e.reciprocal(out=rsn.t[:, :], in_=rsn.t[:, :]), [rsn], [rsn])
        for a in range(4):
            if a % 2 == 0:
                op("dve", lambda e, a=a: e.tensor_scalar(out=hn.t[:, a, :], in0=xt[:, a, :],
                                                         scalar1=rsn.t[:, a:a + 1], scalar2=None, op0=ALU.mult),
                   [BIGlo, rsn], [hn])
            else:
                op("act", lambda e, a=a: e.activation(out=hn.t[:, a, :], in_=xt[:, a, :], func=AF.Copy,
                                                      scale=rsn.t[:, a:a + 1]), [BIGlo, rsn], [hn])
        for c in range(8):
            p = pt()
            for a in range(4):
                op("pe", lambda e, a=a, c=c, p=p: e.transpose(out=p.t[:, a * 128:(a + 1) * 128],
                                                              in_=hn.t[:, a, c * 128:(c + 1) * 128],
                                                              identity=ident.t[:, :]), [hn, ident], [p])
            copy_any(alt("dve", "act"), hT.t[:, c, :], p.t[:, 0:512], [p], [hT])

    def mm_tm(p, a, slot, w, ncols=None, m0=None, m=128):
        ncols = w if ncols is None else ncols
        m0 = a * 128 if m0 is None else m0
        for c in range(8):
            op("pe", lambda e, c=c: e.matmul(p.t[0:m, 0:ncols], lhsT=hT.t[:, c, m0:m0 + m],
                                             rhs=slot.t[:, c * w:c * w + ncols], start=(c == 0), stop=(c == 7)),
               [hT, slot], [p])

    def mm_fm(p, slot, w, col0):
        for c in range(8):
            op("pe", lambda e, c=c: e.matmul(p.t[:, :], lhsT=slot.t[:, c * w + col0:c * w + col0 + 128],
                                             rhs=hT.t[:, c, :], start=(c == 0), stop=(c == 7)), [hT, slot], [p])

    def stage_qkv(l, j):
        slot = w_get(B_AQ)
        for a in range(4):
            p = pg()
            mm_tm(p, a, slot, 512)
            sq = nrt()
            op("act", lambda e, p=p, sq=sq: e.activation(out=sq.t[:, :], in_=p.t[:, :], func=AF.Square), [p], [sq])
            op("dve", lambda e, sq=sq: e.tensor_reduce(out=smq.t[:, :], in_=sq.t[:, :].rearrange("p (h d) -> p h d", d=64),
                                                       axis=AX.X, op=ALU.add), [sq], [smq])
            rstd_inplace(smq, smq.t[:, :], 1.0 / 64)
            op("dve", lambda e, p=p, sq=sq: e.tensor_tensor(
                out=sq.t[:, :].rearrange("p (h d) -> p h d", d=64), in0=p.t[:, :].rearrange("p (h d) -> p h d", d=64),
                in1=bc(smq.t[:, :].unsqueeze(2), [128, 8, 64]), op=ALU.mult), [p, smq], [sq])
            op("pool", lambda e, sq=sq: e.tensor_tensor(
                out=qh.t[:, :, :, :].rearrange("p i g d -> p g i d"),
                in0=sq.t[:, :].rearrange("p (g i d) -> p g i d", g=2, i=4),
                in1=gq_b.t[:, l, :].rearrange("p (g i d) -> p g i d", g=2, i=4), op=ALU.mult), [sq, gq_b], [qh])
            pT = pt()
            for i in range(4):
                op("pe", lambda e, i=i, pT=pT: e.transpose(out=pT.t[:, i * 128:(i + 1) * 128],
                                                           in_=qh.t[:, i, :, :].rearrange("p g d -> p (g d)"),
                                                           identity=ident.t[:, :]), [qh, ident], [pT])
            copy_any("act", qT.t[:, a, :, :], pT.t[:, 0:512].rearrange("p (i q) -> p i q", i=4), [pT], [qT])
        w_issue()
        if int(_os.environ.get("STOPQ", "9")) < 2:
            return
        slot = w_get(B_KVI)
        for a in range(4):
            blk = 4 * j + a
            p = pg()
            mm_tm(p, a, slot, 512)
            sq = nrt()
            op("act", lambda e, p=p, sq=sq: e.activation(out=sq.t[:, 0:128], in_=p.t[:, 0:128], func=AF.Square), [p], [sq])
            op("dve", lambda e, sq=sq: e.tensor_reduce(out=smk.t[:, :], in_=sq.t[:, 0:128].rearrange("p (h d) -> p h d", d=64),
                                                       axis=AX.X, op=ALU.add), [sq], [smk])
            rstd_inplace(smk, smk.t[:, :], 1.0 / 64)
            op("dve", lambda e, p=p, sq=sq: e.tensor_tensor(
                out=sq.t[:, 0:128].rearrange("p (h d) -> p h d", d=64),
                in0=p.t[:, 0:128].rearrange("p (h d) -> p h d", d=64),
                in1=bc(smk.t[:, :].unsqueeze(2), [128, 2, 64]), op=ALU.mult), [p, smk], [sq])
            op("pool", lambda e, sq=sq: e.tensor_tensor(out=kh.t[:, :], in0=sq.t[:, 0:128], in1=gk_b.t[:, l, :],
                                                        op=ALU.mult), [sq, gk_b], [kh])
            copy_any("act", Vaug.t[:, blk, :, 0:64], p.t[:, 128:256].rearrange("p (g d) -> p g d", g=2), [p], [Vaug])
            copy_any("dve", iqb.t[:, :], p.t[:, 256:512], [p], [iqb])
            pT = pt()
            op("pe", lambda e, pT=pT: e.transpose(out=pT.t[:, 0:128], in_=kh.t[:, :], identity=ident.t[:, :]),
               [kh, ident], [pT])
            copy_any("dve", kT.t[:, blk * 128:(blk + 1) * 128], pT.t[:, 0:128], [pT], [kT])
            pT2 = pt()
            for h in range(4):
                op("pe", lambda e, h=h, pT2=pT2: e.transpose(out=pT2.t[0:64, h * 128:(h + 1) * 128],
                                                             in_=iqb.t[:, h * 64:(h + 1) * 64],
                                                             identity=ident.t[:, :]), [iqb, ident], [pT2])
            copy_any("act", qiT.t[:, a, :, :], pT2.t[0:64, 0:512].rearrange("p (h q) -> p h q", h=4), [pT2], [qiT])
        w_issue()
        if int(_os.environ.get("STOPQ", "9")) < 3:
            return
        slot = w_get(B_IKW)
        pk = pg()
        for c in range(8):
            op("pe", lambda e, c=c, pk=pk: e.matmul(pk.t[0:64, :], lhsT=slot.t[:, c * 128:c * 128 + 64], rhs=hT.t[:, c, :],
                                                    start=(c == 0), stop=(c == 7)), [hT, slot], [pk])
        copy_any("act", kiT.t[:, j * 512:(j + 1) * 512], pk.t[0:64, :], [pk], [kiT])
        for a in range(4):
            p = pg()
            mm_tm(p, a, slot, 128)
            op("act", lambda e, p=p, a=a: e.activation(out=aabs.t[:, a, :], in_=p.t[:, 64:68], func=AF.Abs,
                                                       scale=1.0 / 16), [p], [aabs])
            op("dve", lambda e, p=p, a=a: e.tensor_scalar(out=asgn.t[:, a, :], in0=p.t[:, 64:68], scalar1=0.0, scalar2=2.0,
                                                          op0=ALU.is_ge, op1=ALU.mult), [p], [asgn])
            op("dve", lambda e, a=a: e.tensor_scalar(out=asgn.t[:, a, :], in0=asgn.t[:, a, :], scalar1=-1.0, scalar2=None,
                                                     op0=ALU.add), [asgn], [asgn])
        w_issue()

    def stage_attention(l, j, a):
        qb = 4 * j + a
        nch = qb // 4 + 1
        Wd = 512 * nch
        nvalid = 128 * (qb + 1)
        sc = BIG.t
        for ch in range(nch):
            scc = sc[:, ch * 512:(ch + 1) * 512]
            for h in range(4):
                p = pg()
                op("pe", lambda e, p=p, h=h, ch=ch: e.matmul(p.t[:, :], lhsT=qiT.t[:, a, h, :],
                                                             rhs=kiT.t[:, ch * 512:(ch + 1) * 512],
                                                             start=True, stop=True), [qiT, kiT], [p])
                r = nrt()
                op("act", lambda e, p=p, r=r, h=h: e.activation(out=r.t[:, :], in_=p.t[:, :], func=AF.Relu,
                                                                scale=aabs.t[:, a, h:h + 1]), [p, aabs], [r])
                if h == 0:
                    op("dve", lambda e, r=r, scc=scc: e.tensor_scalar(out=scc, in0=r.t[:, :], scalar1=asgn.t[:, a, 0:1],
                                                                      scalar2=None, op0=ALU.mult), [r, asgn], SC)
                else:
                    op("dve", lambda e, r=r, scc=scc, h=h: e.scalar_tensor_tensor(
                        out=scc, in0=r.t[:, :], scalar=asgn.t[:, a, h:h + 1], in1=scc, op0=ALU.mult, op1=ALU.add),
                       [r, asgn] + SC, SC)
        op("dve", lambda e: e.tensor_reduce(out=bis.t[:, 5:6], in_=sc[:, 0:nvalid], axis=AX.X, op=ALU.max,
                                            apply_absolute_value=True), SC, [bis])
        op("dve", lambda e: e.tensor_scalar(out=bis.t[:, 0:1], in0=bis.t[:, 5:6], scalar1=-1.0, scalar2=-1.0,
                                            op0=ALU.mult, op1=ALU.add), [bis], [bis])
        op("dve", lambda e: e.tensor_scalar(out=bis.t[:, 1:2], in0=bis.t[:, 5:6], scalar1=2.0, scalar2=2.0,
                                            op0=ALU.mult, op1=ALU.add), [bis], [bis])
        if nvalid < Wd:
            op("pool", lambda e: e.memset(sc[:, nvalid:Wd], NEG), SC + [bis], SC)
        op("pool", lambda e: e.tensor_tensor(out=sc[:, qb * 128:(qb + 1) * 128], in0=sc[:, qb * 128:(qb + 1) * 128],
                                             in1=negtri.t[:, :], op=ALU.add), SC + [negtri, bis], SC)
        split = nvalid >= 512
        hd = (int(nvalid * 0.47) // 64) * 64 if split else nvalid
        SB_ = 1048576.0
        for k in range(1, NIT + 1):
            f = 2.0 ** (-k)
            op("dve", lambda e, f=f: e.scalar_tensor_tensor(out=bis.t[:, 2:3], in0=bis.t[:, 1:2], scalar=f,
                                                            in1=bis.t[:, 0:1], op0=ALU.mult, op1=ALU.add), [bis], [bis])
            if split:
                op("dve", lambda e: e.tensor_scalar(out=nbv.t[:, 0:1], in0=bis.t[:, 2:3], scalar1=-SB_, scalar2=None,
                                                    op0=ALU.mult), [bis], [nbv])
                op("act", lambda e: e.activation(out=maskT.t[:, hd:nvalid], in_=sc[:, hd:nvalid], func=AF.Sigmoid,
                                                 scale=SB_, bias=nbv.t[:, 0:1], accum_out=bisA.t[:, 0:1]),
                   SC + [nbv], [junkA, bisA])
            op("dve", lambda e: e.tensor_scalar(out=maskT.t[:, 0:hd], in0=sc[:, 0:hd], scalar1=bis.t[:, 2:3],
                                                scalar2=None, op0=ALU.is_ge, op1=ALU.add, accum_out=bis.t[:, 3:4]),
               SC + [bis], [maskT, bis])
            if split:
                op("dve", lambda e: e.tensor_tensor(out=bis.t[:, 3:4], in0=bis.t[:, 3:4], in1=bisA.t[:, 0:1], op=ALU.add),
                   [bis, bisA], [bis])
            op("dve", lambda e: e.tensor_scalar(out=bis.t[:, 4:5], in0=bis.t[:, 3:4], scalar1=255.5,
                                                scalar2=bis.t[:, 1:2], op0=ALU.is_ge, op1=ALU.mult), [bis], [bis])
            op("dve", lambda e, f=f: e.scalar_tensor_tensor(out=bis.t[:, 0:1], in0=bis.t[:, 4:5], scalar=f,
                                                            in1=bis.t[:, 0:1], op0=ALU.mult, op1=ALU.add), [bis], [bis])
        for ch in range(nch):
            m = mrow[ch % 2]
            op("dve", lambda e, m=m, ch=ch: e.tensor_scalar(out=m.t[:, :], in0=sc[:, ch * 512:(ch + 1) * 512],
                                                            scalar1=bis.t[:, 0:1], scalar2=None, op0=ALU.is_ge),
               SC + [bis], [m])
            nk = min(4, qb + 1 - 4 * ch)
            pT = pt()
            for k4 in range(nk):
                op("pe", lambda e, m=m, k4=k4, pT=pT: e.transpose(out=pT.t[:, k4 * 128:(k4 + 1) * 128],
                                                                  in_=m.t[:, k4 * 128:(k4 + 1) * 128],
                                                                  identity=ident.t[:, :]), [m, ident], [pT])
            copy_any("act", maskT.t[:, ch * 512:ch * 512 + nk * 128], pT.t[:, 0:nk * 128], [pT], [maskT, junkA])
        for kb in range(qb + 1):
            kind = qb - kb
            for g in range(2):
                p = pg()
                op("pe", lambda e, p=p, g=g, kb=kb: e.matmul(
                    p.t[:, :], lhsT=kT.t[g * 64:(g + 1) * 64, kb * 128:(kb + 1) * 128],
                    rhs=qT.t[g * 64:(g + 1) * 64, a, :, :].rearrange("p i q -> p (i q)"), start=True, stop=True),
                   [kT, qT], [p])
                E = Eb[g]
                P = Pb[g]
                op("act", lambda e, p=p, E=E: e.activation(out=E.t[:, :], in_=p.t[:, :], func=AF.Exp, scale=0.125),
                   [p], [E])
                E3 = E.t[:, :].rearrange("p (h q) -> p h q", h=4)
                P3 = P.t[:, :].rearrange("p (h q) -> p h q", h=4)
                if kind <= 1:
                    op("pool", lambda e, E3=E3, g=g, kind=kind: e.tensor_tensor(
                        out=E3, in0=E3, in1=TB.t[:, kind, g * 4:(g + 1) * 4, :], op=ALU.mult), [E, TB], [E])
                op("dve" if g == 0 else "pool", lambda e, E3=E3, P3=P3, kb=kb: e.tensor_tensor(
                    out=P3, in0=E3, in1=bc(maskT3[:, kb, :].unsqueeze(1), [128, 4, 128]), op=ALU.mult),
                   [E, maskT], [P])
                for h in range(4):
                    op("pe", lambda e, P=P, h=h, g=g, kb=kb: e.matmul(
                        psO[g].t[:, h * 65:(h + 1) * 65], lhsT=P.t[:, h * 128:(h + 1) * 128],
                        rhs=Vaug.t[:, kb, g, :], start=(kb == 0 and h == 0), stop=(kb == qb),
                        skip_group_check=True), [P, Vaug], [psO[g]])
        for g in range(2):
            o3 = psO[g].t[:, 0:260].rearrange("p (h e) -> p h e", e=65)
            op("dve", lambda e, g=g, o3=o3: e.reciprocal(out=orc.t[:, g * 4:(g + 1) * 4], in_=o3[:, :, 64]),
               [psO[g]], [orc])
            op("dve", lambda e, g=g, o3=o3: e.tensor_tensor(
                out=oev.t[:, g * 4:(g + 1) * 4, :], in0=o3[:, :, 0:64],
                in1=bc(orc.t[:, g * 4:(g + 1) * 4].unsqueeze(2), [128, 4, 64]), op=ALU.mult), [psO[g], orc], [oev])
        pT = pt()
        for fc in range(4):
            op("pe", lambda e, fc=fc, pT=pT: e.transpose(
                out=pT.t[:, fc * 128:(fc + 1) * 128], in_=oev.t[:, 2 * fc:2 * fc + 2, :].rearrange("p h d -> p (h d)"),
                identity=ident.t[:, :]), [oev, ident], [pT])
        copy_any("act", yTa[:, :, a * 128:(a + 1) * 128], pT.t[:, 0:512].rearrange("p (f q) -> p f q", f=4), [pT], [hn])

    def stage_hgrn(l, j):
        slot = w_get(B_HI)
        for c8 in range(8):
            p = pg()
            mm_tm(p, None, slot, 512, m0=c8 * 64, m=64)
            copy_any(alt("act", "dve"), i64[:, c8, :], p.t[0:64, :], [p], [maskT])
        w_issue()
        slot = w_get(B_HG)
        for c8 in range(8):
            p = pg()
            mm_tm(p, None, slot, 512, m0=c8 * 64, m=64)
            r = nrt()
            op("act", lambda e, p=p, r=r: e.activation(out=r.t[0:64, :], in_=p.t[0:64, :], func=AF.Silu), [p], [r])
            op("pool", lambda e, r=r, c8=c8: e.tensor_tensor(out=y64[:, c8, :], in0=r.t[0:64, :], in1=gn_b.t[0:64, l, :],
                                                             op=ALU.mult), [r, gn_b], [maskT])
        w_issue()
        slot = w_get(B_HQ)
        for hh in range(4):
            p = pg()
            mm_fm(p, slot, 512, hh * 128)
            copy_any("act", Ti(hh), p.t[:, :], [p], [BIGhi])
        w_issue()
        slot = w_get(B_HF)
        T4, T5, T6, T7 = Ti(4), Ti(5), Ti(6), Ti(7)
        Ut = Ti(4, 2).rearrange("p (c v) -> p c v", c=8)
        sqv = Ti(6, 2)
        for hh in range(4):
            p = pg()
            mm_fm(p, slot, 512, hh * 128)
            op("act", lambda e, p=p: e.activation(out=T4, in_=p.t[:, :], func=AF.Sigmoid), [p], [BIGhi])
            op("act", lambda e, hh=hh: e.activation(out=T5, in_=T4, func=AF.Ln, scale=oml.t[:, l, hh:hh + 1],
                                                    bias=lbv.t[:, l, hh:hh + 1]), [BIGhi, oml, lbv], [BIGhi])
            op("dve", lambda e, hh=hh: e.tensor_scalar(out=T4, in0=T4, scalar1=noml.t[:, l, hh:hh + 1],
                                                       scalar2=oml.t[:, l, hh:hh + 1], op0=ALU.mult, op1=ALU.add),
               [BIGhi, noml, oml], [BIGhi])
            op("dve", lambda e: e.tensor_tensor_scan(out=T6, data0=rmask.t[:, :], data1=T5, initial=0.0,
                                                     op0=ALU.mult, op1=ALU.add), [BIGhi, rmask], [BIGhi])
            G3 = T6.rearrange("p (c t) -> p c t", t=64)
            op("dve", lambda e, G3=G3: e.tensor_tensor(out=T5.rearrange("p (c t) -> p c t", t=64), in0=G3,
                                                       in1=bc(G3[:, :, 31:32], [128, 8, 64]), op=ALU.subtract),
               [BIGhi], [BIGhi])
            op("act", lambda e: e.activation(out=T7, in_=T5, func=AF.Exp), [BIGhi], [BIGhi])
            op("act", lambda e: e.activation(out=T5, in_=T5, func=AF.Exp, scale=-1.0), [BIGhi], [BIGhi])
            op("act", lambda e, G3=G3: e.activation(out=hsm.t[:, 0:8], in_=G3[:, :, 31], func=AF.Exp), [BIGhi], [hsm])
            op("act", lambda e, G3=G3: e.activation(out=hsm.t[:, 8:16], in_=G3[:, :, 63], func=AF.Exp), [BIGhi], [hsm])
            op("dve", lambda e, hh=hh: e.tensor_tensor(out=qgb.t[:, :], in0=Ti(hh), in1=T7, op=ALU.mult), [BIGhi], [qgb])
            op("pool", lambda e: e.tensor_tensor(out=kgb.t[:, :], in0=T4, in1=T5, op=ALU.mult), [BIGhi], [kgb])
            pA = pg()
            for c8 in range(8):
                op("pe", lambda e, c8=c8, pA=pA: e.matmul(pA.t[0:64, c8 * 64:(c8 + 1) * 64],
                                                          lhsT=kgb.t[:, c8 * 64:(c8 + 1) * 64],
                                                          rhs=qgb.t[:, c8 * 64:(c8 + 1) * 64], start=True, stop=True,
                                                          skip_group_check=True), [kgb, qgb], [pA])
            op("dve", lambda e, pA=pA: e.tensor_tensor(out=ATb.t[:, :, :],
                                                       in0=pA.t[0:64, :].rearrange("p (c t) -> p c t", t=64),
                                                       in1=bc(tri64.t[:, :].unsqueeze(1), [64, 8, 64]), op=ALU.mult),
               [pA, tri64], [ATb])
            pK = pt()
            for c8 in range(8):
                op("pe", lambda e, c8=c8, pK=pK: e.transpose(out=pK.t[0:64, c8 * 128:(c8 + 1) * 128],
                                                             in_=kgb.t[:, c8 * 64:(c8 + 1) * 64],
                                                             identity=ident.t[:, :]), [kgb, ident], [pK])
            copy_any("act", kgT.t[:, :, :], pK.t[0:64, :].rearrange("p (c d) -> p c d", c=8), [pK], [kgT])
            for c8 in range(8):
                op("pe", lambda e, c8=c8, hh=hh: e.matmul(psO[c8 // 4].t[:, (c8 % 4) * 128:(c8 % 4 + 1) * 128],
                                                          lhsT=kgT.t[:, c8, :], rhs=i64[:, c8, hh * 128:(hh + 1) * 128],
                                                          start=True, stop=True, skip_group_check=True),
                   [kgT, maskT], [psO[c8 // 4]])
            E63 = T7.rearrange("p (c t) -> p c t", t=64)
            for k in range(2):
                op("dve", lambda e, k=k, E63=E63: e.tensor_tensor(
                    out=Ut[:, 4 * k:4 * k + 4, :], in0=psO[k].t[:, :].rearrange("p (c v) -> p c v", c=4),
                    in1=bc(E63[:, 4 * k:4 * k + 4, 63:64], [128, 4, 128]), op=ALU.mult), [psO[k], BIGhi], [BIGhi])
            copy_any("pool", Shist.t[:, 0, :], Scar.t[:, hh, :], [Scar], [Shist])
            for c8 in range(8):
                op("dve", lambda e, c8=c8: e.scalar_tensor_tensor(out=Shist.t[:, c8 + 1, :], in0=Shist.t[:, c8, :],
                                                                  scalar=hsm.t[:, 8 + c8:9 + c8], in1=Ut[:, c8, :],
                                                                  op0=ALU.mult, op1=ALU.add), [Shist, hsm, BIGhi], [Shist])
            copy_any("pool", Scar.t[:, hh, :], Shist.t[:, 8, :], [Shist], [Scar])
            op("pool", lambda e: e.tensor_tensor(out=stp.t[:, :, :], in0=Shist.t[:, 0:8, :],
                                                 in1=bc(hsm.t[:, 0:8].unsqueeze(2), [128, 8, 128]), op=ALU.mult),
               [Shist, hsm], [stp])
            for c8 in range(8):
                reg = psO[c8 // 4].t[0:64, (c8 % 4) * 128:(c8 % 4 + 1) * 128]
                op("pe", lambda e, c8=c8, reg=reg, hh=hh: e.matmul(reg, lhsT=ATb.t[:, c8, :],
                                                                   rhs=i64[:, c8, hh * 128:(hh + 1) * 128],
                                                                   start=True, stop=False, skip_group_check=True),
                   [ATb, maskT], [psO[c8 // 4]])
                op("pe", lambda e, c8=c8, reg=reg: e.matmul(reg, lhsT=qgb.t[:, c8 * 64:(c8 + 1) * 64],
                                                            rhs=stp.t[:, c8, :], start=False, stop=True,
                                                            skip_group_check=True), [qgb, stp], [psO[c8 // 4]])
            for k in range(2):
                op("act", lambda e, k=k: e.activation(out=sqv[0:64, k * 512:(k + 1) * 512], in_=psO[k].t[0:64, :],
                                                      func=AF.Square), [psO[k]], [BIGhi])
            op("dve", lambda e: e.tensor_reduce(out=smo.t[0:64, :], in_=sqv[0:64, :].rearrange("p (c v) -> p c v", v=128),
                                                axis=AX.X, op=ALU.add), [BIGhi], [smo])
            rstd_inplace(smo, smo.t[0:64, :], 1.0 / 128)
            for k in range(2):
                op("dve", lambda e, k=k: e.tensor_tensor(
                    out=sqv[0:64, k * 512:(k + 1) * 512].rearrange("p (c v) -> p c v", c=4),
                    in0=psO[k].t[0:64, :].rearrange("p (c v) -> p c v", c=4),
                    in1=bc(smo.t[0:64, 4 * k:4 * k + 4].unsqueeze(2), [64, 4, 128]), op=ALU.mult),
                   [psO[k], smo, BIGhi], [BIGhi])
            op("pool", lambda e, hh=hh: e.tensor_tensor(out=y64[:, :, hh * 128:(hh + 1) * 128],
                                                        in0=sqv[0:64, :].rearrange("p (c v) -> p c v", v=128),
                                                        in1=y64[:, :, hh * 128:(hh + 1) * 128], op=ALU.mult),
               [BIGhi, maskT], [maskT])
        w_issue()
        for fc in range(4):
            pT = pt()
            for c8 in range(8):
                op("pe", lambda e, c8=c8, fc=fc, pT=pT: e.transpose(out=pT.t[:, c8 * 64:(c8 + 1) * 64],
                                                                    in_=y64[:, c8, fc * 128:(fc + 1) * 128],
                                                                    identity=ident.t[0:64, 0:64]), [maskT, ident], [pT])
            copy_any(alt("act", "dve"), yTb[:, fc, :], pT.t[:, 0:512], [pT], [hn])

    def stage_merge(l, bg, bu, yT, mode):
        gs = [w_get(bg), w_get(bg + 1)]
        us = w_get(bu)
        for mc in range(8):
            pgate = pg()
            mm_fm(pgate, gs[mc // 4], 512, (mc % 4) * 128)
            pU = pg()
            for fc in range(4):
                op("pe", lambda e, fc=fc, mc=mc, pU=pU: e.matmul(pU.t[:, :],
                                                                 lhsT=us.t[:, fc * 1024 + mc * 128:fc * 1024 + (mc + 1) * 128],
                                                                 rhs=yT[:, fc, :], start=(fc == 0), stop=(fc == 3)),
                   [us, hn], [pU])
            sg = nrt()
            op("act", lambda e, pgate=pgate, sg=sg: e.activation(out=sg.t[:, :], in_=pgate.t[:, :], func=AF.Sigmoid),
               [pgate], [sg])
            if mode == "first":
                op("dve", lambda e, sg=sg, pU=pU, mc=mc: e.tensor_tensor(out=mixT[:, mc, :], in0=pU.t[:, :], in1=sg.t[:, :],
                                                                         op=ALU.mult), [pU, sg], [BIGhi])
            else:
                op("dve", lambda e, sg=sg, pU=pU: e.tensor_tensor(out=sg.t[:, :], in0=pU.t[:, :], in1=sg.t[:, :],
                                                                  op=ALU.mult), [pU, sg], [sg])
                if mode == "acc":
                    op("pool", lambda e, sg=sg, mc=mc: e.tensor_tensor(out=mixT[:, mc, :], in0=mixT[:, mc, :],
                                                                       in1=sg.t[:, :], op=ALU.add), [sg, BIGhi], [BIGhi])
                else:
                    op("pool", lambda e, sg=sg, mc=mc: e.tensor_tensor(out=mixTb[:, mc, :], in0=mixT[:, mc, :],
                                                                       in1=sg.t[:, :], op=ALU.add), [sg, BIGhi], [maskT])
        w_issue()
        w_issue()
        w_issue()

    def stage_conv(l, j):
        sb_ = w_get(B_CB)
        sc_ = w_get(B_CC)
        sh_ = w_get(B_CH)
        for cc in range(4):
            pB, pC, pH = pg(), pg(), pg()
            mm_fm(pB, sb_, 512, cc * 128)
            mm_fm(pC, sc_, 512, cc * 128)
            mm_fm(pH, sh_, 512, cc * 128)
            r0 = nrt()
            copy_any("act", r0.t[:, :], pC.t[:, :], [pC], [r0])
            op("dve", lambda e, cc=cc, r0=r0, pH=pH: e.tensor_tensor(out=ubuf.t[:, cc, 2:514], in0=pH.t[:, :],
                                                                     in1=r0.t[:, :], op=ALU.mult), [pH, r0], [ubuf])
            r1 = nrt()
            op("dve", lambda e, cc=cc, r1=r1: e.tensor_scalar(out=r1.t[:, :], in0=ubuf.t[:, cc, 2:514],
                                                              scalar1=cw.t[:, l, cc, 2:3], scalar2=None, op0=ALU.mult),
               [ubuf, cw], [r1])
            op("dve", lambda e, cc=cc, r1=r1: e.scalar_tensor_tensor(out=r1.t[:, :], in0=ubuf.t[:, cc, 1:513],
                                                                     scalar=cw.t[:, l, cc, 1:2], in1=r1.t[:, :],
                                                                     op0=ALU.mult, op1=ALU.add), [ubuf, cw, r1], [r1])
            op("dve", lambda e, cc=cc, r1=r1: e.scalar_tensor_tensor(out=r1.t[:, :], in0=ubuf.t[:, cc, 0:512],
                                                                     scalar=cw.t[:, l, cc, 0:1], in1=r1.t[:, :],
                                                                     op0=ALU.mult, op1=ALU.add), [ubuf, cw, r1], [r1])
            op("dve", lambda e, cc=cc, r1=r1, pB=pB: e.tensor_tensor(out=yTb[:, cc, :], in0=pB.t[:, :], in1=r1.t[:, :],
                                                                     op=ALU.mult), [pB, r1], [hn])
            copy_any("pool", ubuf.t[:, cc, 0:2], ubuf.t[:, cc, 512:514], [ubuf], [ubuf])
        w_issue()
        w_issue()
        w_issue()

    def stage_wout(l):
        for half in range(2):
            slot = w_get(B_WO + half)
            for a in range(4):
                p = pg()
                for mc in range(8):
                    op("pe", lambda e, mc=mc, a=a, p=p, slot=slot: e.matmul(
                        p.t[:, :], lhsT=mixTb[:, mc, a * 128:(a + 1) * 128], rhs=slot.t[:, mc * 512:(mc + 1) * 512],
                        start=(mc == 0), stop=(mc == 7)), [maskT, slot], [p])
                xs = xt[:, a, half * 512:(half + 1) * 512]
                op("dve", lambda e, p=p, xs=xs: e.tensor_tensor(out=xs, in0=p.t[:, :], in1=xs, op=ALU.add),
                   [p, BIGlo], [BIGlo])
            w_issue()

    def stage_mlp(l):
        for g in range(8):
            su = w_get(B_MLP + 2 * g)
            for f4 in range(4):
                p = pg()
                mm_fm(p, su, 512, f4 * 128)
                rb_ = Eb[f4 % 2]
                op("act", lambda e, p=p, rb_=rb_: e.activation(out=rb_.t[:, :], in_=p.t[:, :], func=AF.Relu), [p], [rb_])
                op("pool", lambda e, rb_=rb_, f4=f4: e.tensor_tensor(out=uT[:, f4, :], in0=rb_.t[:, :], in1=rb_.t[:, :],
                                                                     op=ALU.mult), [rb_], [qT])
            w_issue()
            sd = w_get(B_MLP + 2 * g + 1)
            for a in range(4):
                for half in range(2):
                    p = pg()
                    for f4 in range(4):
                        op("pe", lambda e, f4=f4, a=a, half=half, p=p, sd=sd: e.matmul(
                            p.t[:, :], lhsT=uT[:, f4, a * 128:(a + 1) * 128],
                            rhs=sd.t[:, f4 * 1024 + half * 512:f4 * 1024 + (half + 1) * 512],
                            start=(f4 == 0), stop=(f4 == 3)), [qT, sd], [p])
                    xs = xt[:, a, half * 512:(half + 1) * 512]
                    op("dve", lambda e, p=p, xs=xs: e.tensor_tensor(out=xs, in0=p.t[:, :], in1=xs, op=ALU.add),
                       [p, BIGlo], [BIGlo])
            w_issue()

    for li, l in enumerate(layers):
        xsrc = x_in if li == 0 else xmid[li - 1]
        xsrc_tok = [] if li == 0 else [xmid_tok[li - 1]]
        xdst = out_d if li == NL - 1 else xmid[li]
        xdst_tok = [xmid_tok[0]] if NL > 1 else []
        op("pool", lambda e: e.memset(Scar.t[:, :, :], 0.0), [], [Scar])
        op("pool", lambda e: e.memset(ubuf.t[:, :, :], 0.0), [], [ubuf])
        for j in range(NT):
            xs_ap = xsrc[j * 512:(j + 1) * 512, :].rearrange("(a p) d -> p a d", p=128)
            xd_ap = xdst[j * 512:(j + 1) * 512, :].rearrange("(a p) d -> p a d", p=128)
            K.dma("sp", xt, xs_ap, xsrc_tok, [BIGlo])
            if upto >= 2:
                rms_to_hT()
                dump("hT", hT.t[:, :, :].rearrange("p c t -> p (c t)"), hT)
            if upto >= 3:
                stage_qkv(l, j)
                dump("qT", qT.t[:, :, :, :].rearrange("p a i q -> p (a i q)"), qT)
                dump("kT", kT.t[:, 0:512], kT)
            if upto >= 4:
                for a in range(4):
                    stage_attention(l, j, a)
                    if a == 0:
                        dump("sc", BIG.t[:, 0:512], BIGlo)
                        dump("bis", bis.t[:, :], bis)
                dump("yTa", hn.t[:, :, :].rearrange("p a t -> p (a t)"), hn)
            if upto >= 5:
                K.dma("sp", xt, xs_ap, xsrc_tok, [BIGlo])
                stage_hgrn(l, j)
                dump("yTb", hn.t[:, :, :].rearrange("p a t -> p (a t)"), hn)
            if upto >= 6:
                stage_merge(l, B_GH, B_UH, yTb, "first")
                stage_conv(l, j)
                stage_merge(l, B_GC, B_UC, yTb, "acc")
                stage_merge(l, B_GA, B_UA, yTa, "last")
                stage_wout(l)
                rms_to_hT()
                stage_mlp(l)
            K.dma("sp", xd_ap, xt, [BIGlo], xdst_tok, sem_tok=BIGlo, final=(li == NL - 1))
            if upto < 6:
                break
    K.finish()
    return nc, K


FUSED = True
SEQ_FULL = 8192


def _run(S, layers, xs, weights, NIT=17, dbg=None, trace=False, upto=99):
    nc, K = build_program(S, layers, NIT=NIT, dbg=dbg, upto=upto)
    ohc = onehot_table()
    in_maps = []
    for x in xs:
        m = {"x": np.ascontiguousarray(x, dtype=np.float32), "ohc": ohc}
        for n in WNAMES:
            m[n] = np.ascontiguousarray(weights[n], dtype=np.float32)
        in_maps.append(m)
    res = run_bass_kernel_spmd(nc, in_maps, core_ids=list(range(len(xs))), **({"trace": True} if trace else {}))
    return res


def kernel(**inputs):
    x = np.asarray(inputs["x"], dtype=np.float32)
    B, S, _ = x.shape
    weights = {n: np.asarray(inputs[n], dtype=np.float32) for n in WNAMES}
    xs = [x[b] for b in range(B)]
    if FUSED:
        res = _run(S, [0, 1], xs, weights)
        return np.stack([r["out"] for r in res.results], axis=0).astype(np.float32)
    for l in range(2):
        res = _run(S, [l], xs, weights)
        xs = [r["out"] for r in res.results]
    return np.stack(xs, axis=0).astype(np.float32)
```
